# Optimizing a Trainium2 kernel written in Bass

```python
import math
import jax, jax.numpy as jnp
from jax import lax
import numpy as np

D_MODEL = 1024
BATCH = 4
SEQ = 4096
DEPTH = 2

D_MIX = D_MODEL
N_MIXERS = 4
D_GROUP = D_MIX // N_MIXERS
HEADS_PER_MIXER = 4
HEAD_DIM = D_GROUP // HEADS_PER_MIXER
CHUNK = 128
CONF_KERNEL = 31
POOL_WINDOWS = (2, 4, 8, 16)
SHORT_KERNEL = 3
N_EXPERTS = 16
N_EXPERT_GROUPS = 4
EXPERTS_PER_GROUP = N_EXPERTS // N_EXPERT_GROUPS
TOP_K = 2
D_EXPERT = 512
D_IN_GMLP = 2 * D_GROUP
D_IN_CONF = 2 * D_GROUP
D_IN_POOL = D_GROUP
D_IN_SCONV = 3 * D_GROUP
D_IN = D_IN_GMLP + D_IN_CONF + D_IN_POOL + D_IN_SCONV
ALPHA = (2 * DEPTH) ** 0.25
BETA = (8 * DEPTH) ** -0.25
LN_EPS = 1e-5
RMS_EPS = 1e-6

kernel_name = "hybrid_gmlp_conformer_pool_shortconv_sharedrouter_moe"


def layer_norm(x, g, b):
    xf = x.astype(jnp.float32)
    mu = jnp.mean(xf, -1, keepdims=True)
    var = jnp.mean(jnp.square(xf - mu), -1, keepdims=True)
    y = (xf - mu) * lax.rsqrt(var + LN_EPS)
    return (y * g.astype(jnp.float32) + b.astype(jnp.float32)).astype(x.dtype)


def group_rms_norm(y, g):
    B_, S_, _ = y.shape
    yf = y.astype(jnp.float32).reshape(B_, S_, N_MIXERS, D_GROUP)
    yf = yf * lax.rsqrt(jnp.mean(jnp.square(yf), -1, keepdims=True) + RMS_EPS)
    return (yf.reshape(B_, S_, D_MIX) * g.astype(jnp.float32)).astype(y.dtype)


def causal_depthwise_conv(x, w):
    K, C = w.shape
    return lax.conv_general_dilated(
        x, w[:, None, :].astype(x.dtype), window_strides=(1,),
        padding=[(K - 1, 0)], dimension_numbers=('NWC', 'WIO', 'NWC'),
        feature_group_count=C)


def spatial_gating_mixer(z, ln_g, ln_b, w_s, b_s):
    u, v = jnp.split(z, 2, axis=-1)
    v = layer_norm(v, ln_g, ln_b)
    B_, S_, _ = v.shape
    v = v.reshape(B_, S_ // CHUNK, CHUNK, HEADS_PER_MIXER, HEAD_DIM)
    mask = jnp.tril(jnp.ones((CHUNK, CHUNK), dtype=bool))
    w = jnp.where(mask, w_s, 0).astype(v.dtype)
    s = jnp.einsum('hts,bnshd->bnthd', w, v) + b_s.T[:, :, None].astype(v.dtype)
    return u * s.reshape(B_, S_, D_GROUP)


def conformer_conv_mixer(z, dw_w, dw_b, ln_g, ln_b, pw):
    a, g = jnp.split(z, 2, axis=-1)
    h = a * jax.nn.sigmoid(g)
    h = causal_depthwise_conv(h, dw_w) + dw_b.astype(h.dtype)
    h = jax.nn.silu(layer_norm(h, ln_g, ln_b))
    return h @ pw


def multiscale_pool_mixer(z, w_grp, scale):
    B_, S_, _ = z.shape
    G = len(POOL_WINDOWS)
    zg = z.astype(jnp.float32).reshape(B_, S_, G, HEAD_DIM)
    csum = jnp.pad(jnp.cumsum(zg, axis=1), ((0, 0), (1, 0), (0, 0), (0, 0)))
    windows = jnp.array(POOL_WINDOWS, dtype=jnp.int32)
    t = jnp.arange(S_, dtype=jnp.int32)[:, None]
    lo_idx = jnp.maximum(t + 1 - windows, 0)
    window_sum = csum[:, 1:] - csum[:, lo_idx, jnp.arange(G)]
    count = jnp.minimum(t + 1, windows).astype(jnp.float32)
    pooled = (window_sum / count[None, :, :, None] - zg).astype(z.dtype)
    y = jnp.einsum('bsgd,gde->bsge', pooled, w_grp).reshape(B_, S_, D_GROUP)
    return y * scale


def short_gated_conv_mixer(z, conv_w):
    b_gate, c_gate, h = jnp.split(z, 3, axis=-1)
    return b_gate * causal_depthwise_conv(c_gate * h, conv_w)


def hybrid_mixer(x, w_in, gm_ln_g, gm_ln_b, gm_w_s, gm_b_s, cf_dw_w, cf_dw_b,
                 cf_ln_g, cf_ln_b, cf_pw, pool_w, pool_scale, sc_w, mix_norm_g, w_o):
    z = x @ w_in
    o1 = D_IN_GMLP
    o2 = o1 + D_IN_CONF
    o3 = o2 + D_IN_POOL
    z_a, z_b, z_c, z_d = z[..., :o1], z[..., o1:o2], z[..., o2:o3], z[..., o3:]
    y = jnp.concatenate([
        spatial_gating_mixer(z_a, gm_ln_g, gm_ln_b, gm_w_s, gm_b_s),
        conformer_conv_mixer(z_b, cf_dw_w, cf_dw_b, cf_ln_g, cf_ln_b, cf_pw),
        multiscale_pool_mixer(z_c, pool_w, pool_scale),
        short_gated_conv_mixer(z_d, sc_w),
    ], axis=-1)
    return group_rms_norm(y, mix_norm_g) @ w_o


def grouped_moe(x, router_w, router_b, w_gate, w_up, w_down):
    B_, S_, D = x.shape
    xt = x.reshape(-1, D)
    logits = (xt @ router_w).astype(jnp.float32)
    sel = logits + router_b.astype(jnp.float32)
    grp = sel.reshape(-1, N_EXPERT_GROUPS, EXPERTS_PER_GROUP)
    grp_score = lax.top_k(grp, TOP_K)[0].sum(-1)
    best = jnp.argmax(grp_score, axis=-1)
    in_group = (jnp.arange(N_EXPERTS) // EXPERTS_PER_GROUP)[None, :] == best[:, None]
    masked = jnp.where(in_group, sel, -jnp.inf)
    _, idx = lax.top_k(masked, TOP_K)
    gates = jax.nn.softmax(jnp.take_along_axis(logits, idx, axis=-1), axis=-1)
    dense_gates = (jax.nn.one_hot(idx, N_EXPERTS, dtype=jnp.float32) * gates[..., None]).sum(1)
    y = jnp.zeros(xt.shape, jnp.float32)
    for e in range(N_EXPERTS):
        h = jax.nn.silu(xt @ w_gate[e]) * (xt @ w_up[e])
        y = y + dense_gates[:, e:e + 1] * (h @ w_down[e]).astype(jnp.float32)
    return y.astype(x.dtype).reshape(B_, S_, D)


def setup_inputs(seed: int = 0) -> dict:
    key = jax.random.key(seed)
    ks = jax.random.split(key, 24)
    L = DEPTH

    def nrm(k, shape, scale):
        return jax.random.normal(k, shape, jnp.float32) * scale

    return {
        "x": nrm(ks[0], (BATCH, SEQ, D_MODEL), 1.0),
        "w_in": nrm(ks[1], (L, D_MODEL, D_IN), D_MODEL ** -0.5),
        "gm_ln_g": 1.0 + nrm(ks[2], (L, D_GROUP), 0.02),
        "gm_ln_b": nrm(ks[3], (L, D_GROUP), 0.02),
        "gm_w_s": nrm(ks[4], (L, HEADS_PER_MIXER, CHUNK, CHUNK), CHUNK ** -0.5),
        "gm_b_s": 1.0 + nrm(ks[5], (L, HEADS_PER_MIXER, CHUNK), 0.02),
        "cf_dw_w": nrm(ks[6], (L, CONF_KERNEL, D_GROUP), CONF_KERNEL ** -0.5),
        "cf_dw_b": nrm(ks[7], (L, D_GROUP), 0.02),
        "cf_ln_g": 1.0 + nrm(ks[8], (L, D_GROUP), 0.02),
        "cf_ln_b": nrm(ks[9], (L, D_GROUP), 0.02),
        "cf_pw": nrm(ks[10], (L, D_GROUP, D_GROUP), D_GROUP ** -0.5),
        "pool_w": nrm(ks[11], (L, len(POOL_WINDOWS), HEAD_DIM, HEAD_DIM), HEAD_DIM ** -0.5),
        "pool_scale": 1.0 + nrm(ks[12], (L, D_GROUP), 0.02),
        "sc_w": nrm(ks[13], (L, SHORT_KERNEL, D_GROUP), SHORT_KERNEL ** -0.5),
        "mix_norm_g": 1.0 + nrm(ks[14], (L, D_MIX), 0.02),
        "w_o": nrm(ks[15], (L, D_MIX, D_MODEL), BETA * D_MIX ** -0.5),
        "ln1_g": 1.0 + nrm(ks[16], (L, D_MODEL), 0.02),
        "ln1_b": nrm(ks[17], (L, D_MODEL), 0.02),
        "router_w": nrm(ks[18], (D_MODEL, N_EXPERTS), D_MODEL ** -0.5),
        "router_b": nrm(ks[19], (N_EXPERTS,), 0.01),
        "exp_w_gate": nrm(ks[20], (L, N_EXPERTS, D_MODEL, D_EXPERT), D_MODEL ** -0.5),
        "exp_w_up": nrm(ks[21], (L, N_EXPERTS, D_MODEL, D_EXPERT), D_MODEL ** -0.5),
        "exp_w_down": nrm(ks[22], (L, N_EXPERTS, D_EXPERT, D_MODEL), BETA * D_EXPERT ** -0.5),
        "ln2_g": 1.0 + nrm(ks[23], (L, D_MODEL), 0.02),
        "ln2_b": nrm(jax.random.fold_in(ks[23], 1), (L, D_MODEL), 0.02),
    }


def reference(x, w_in, gm_ln_g, gm_ln_b, gm_w_s, gm_b_s, cf_dw_w, cf_dw_b, cf_ln_g,
              cf_ln_b, cf_pw, pool_w, pool_scale, sc_w, mix_norm_g, w_o, ln1_g, ln1_b,
              router_w, router_b, exp_w_gate, exp_w_up, exp_w_down, ln2_g, ln2_b):
    for l in range(DEPTH):
        m = hybrid_mixer(x, w_in[l], gm_ln_g[l], gm_ln_b[l], gm_w_s[l], gm_b_s[l],
                         cf_dw_w[l], cf_dw_b[l], cf_ln_g[l], cf_ln_b[l], cf_pw[l],
                         pool_w[l], pool_scale[l], sc_w[l], mix_norm_g[l], w_o[l])
        x = layer_norm(ALPHA * x + m, ln1_g[l], ln1_b[l])
        f = grouped_moe(x, router_w, router_b, exp_w_gate[l], exp_w_up[l], exp_w_down[l])
        x = layer_norm(ALPHA * x + f, ln2_g[l], ln2_b[l])
    return x
```

```python
from contextlib import ExitStack
import numpy as np
import concourse.bass as bass
import concourse.mybir as mybir
from concourse.bass_utils import run_bass_kernel_spmd

F32 = mybir.dt.float32
BF16 = mybir.dt.bfloat16
ALU = mybir.AluOpType
AF = mybir.ActivationFunctionType
AX = mybir.AxisListType
_DSZ = {F32: 4, BF16: 2}

L = 2
D = 1024
NCORES = 8
SEQ = 4096
TM = 2048
HALO = 128
T = TM + HALO
NT = T // 128
NE = 16
DE = 512
ALPHA = float((2 * L) ** 0.25)
LN_EPS = 1e-5
RMS_EPS = 1e-6
NB = 512
XNB = 512
XBLOCKS = [(0, 128)] + [(128 + i * XNB, XNB) for i in range(TM // XNB)]
MBLOCKS = [(0, 128)] + [(128 + i * NB, NB) for i in range(TM // NB)]
XOFF = []
_o = 0
for (_t0, _nb) in XBLOCKS:
    XOFF.append(_o)
    _o += 8 * _nb
XT_COLS = _o
NPP = 86
PP_DW = 0
PP_DWB = 62
PP_CLG = 64
PP_CLB = 66
PP_PSC = 68
PP_SCW = 70
PP_MG = 76
PP_BS = 82


class Ref:
    __slots__ = ("ap", "buf", "box")

    def __init__(self, ap, buf, box):
        self.ap = ap
        self.buf = buf
        self.box = box

    def v(self, fn):
        return Ref(fn(self.ap), self.buf, self.box)


class Buf:
    def __init__(self, handle, name, P, F, dtype):
        self.h = handle
        self.name = name
        self.P = P
        self.F = F
        self.dtype = dtype
        self.esz = _DSZ[dtype]
        self.entries = {}
        self.whole = False

    def __call__(self, f0=0, f1=None, p0=0, p1=None):
        if f1 is None:
            f1 = self.F
        if p1 is None:
            p1 = self.P
        assert 0 <= f0 < f1 <= self.F and 0 <= p0 < p1 <= self.P, (self.name, f0, f1, p0, p1)
        if self.whole:
            return Ref(self.h[p0:p1, f0:f1], self, (0, self.P, 0, self.F * self.esz))
        return Ref(self.h[p0:p1, f0:f1], self, (p0, p1, f0 * self.esz, f1 * self.esz))


def _ov(a, b):
    return a[0] < b[1] and b[0] < a[1] and a[2] < b[3] and b[2] < a[3]


def _cov(o, i):
    return o[0] <= i[0] and i[1] <= o[1] and o[2] <= i[2] and i[3] <= o[3]


class Eng:
    def __init__(self, name, eng):
        self.name = name
        self.e = eng
        self.sem = name
        self.ticket = 0
        self.seen = {}


class Emitter:
    def __init__(self, nc, sems):
        self.nc = nc
        self.sems = sems
        self.pe = Eng("pe", nc.tensor)
        self.act = Eng("act", nc.scalar)
        self.dve = Eng("dve", nc.vector)
        self.pool = Eng("pool", nc.gpsimd)
        self.sp = Eng("sp", nc.sync)
        self.dma_count = {}
        self.n_inst = 0
        self.n_wait = 0
        self.rec = None

    def _collect(self, E, reads, writes, is_dma):
        need = {}
        for r in reads:
            rb = r.box
            for (box, sk, w), val in r.buf.entries.items():
                if w and _ov(box, rb) and need.get(sk, 0) < val:
                    need[sk] = val
        for wr in writes:
            wb = wr.box
            for (box, sk, w), val in wr.buf.entries.items():
                if _ov(box, wb):
                    if sk == E.sem and E.name == "pe" and not is_dma:
                        continue
                    if need.get(sk, 0) < val:
                        need[sk] = val
        return need

    def _waits(self, E, need):
        for sk, val in need.items():
            if E.seen.get(sk, 0) >= val:
                continue
            E.e.wait_ge(self.sems[sk], val)
            E.seen[sk] = val
            self.n_wait += 1

    def _record(self, reads, writes, sk, val):
        for wr in writes:
            ent = wr.buf.entries
            wb = wr.box
            dead = [k for k in ent if _cov(wb, k[0])]
            for k in dead:
                del ent[k]
            ent[(wb, sk, True)] = val
        for r in reads:
            r.buf.entries[(r.box, sk, False)] = val

    def replay(self, rec):
        self.op(rec[0], rec[1], rec[2], rec[3])

    def op(self, E, fn, w=(), r=()):
        if self.rec is not None:
            self.rec.append((E, fn, tuple(w), tuple(r)))
            return None
        self._waits(E, self._collect(E, r, w, False))
        ins = fn(E.e)
        E.ticket += 1
        ins.then_inc(self.sems[E.sem], 1)
        self._record(r, w, E.sem, E.ticket)
        self.n_inst += 1
        return ins

    def dma(self, E, out, in_, semkey, w=(), r=(), group_left=0):
        self._waits(E, self._collect(E, r, w, True))
        ins = E.e.dma_start(out=out, in_=in_)
        c = self.dma_count.get(semkey, 0) + 16
        self.dma_count[semkey] = c
        ins.then_inc(self.sems[semkey], 16)
        self._record(r, w, semkey, c + 16 * group_left)
        self.n_inst += 1
        return ins


def build(n_layers=L, stop=None):
    nc = bass.Bass("TRN2", target_bir_lowering=False)

    def din(name, shape):
        return nc.dram_tensor(name, list(shape), F32, kind="ExternalInput").ap()

    x_d = din("x", [T, D])
    ident_d = din("ident", [128, 128])
    maskT_d = din("maskT", [128, 128])
    rcfix_d = din("rcfix", [128, 32])
    invw_d = din("invw", [128, 2])
    hflag_d = din("hflag", [128, 1])
    rbb_d = din("rbb", [128, NE])
    pp_d = din("pp", [L, 128, NPP])
    bc3_d = din("bc3", [L, 128, 768])
    lnp_d = din("lnp", [L, 128, 4096])
    gmwT_d = din("gmwT", [L, 128, 512])
    pbd_d = din("pbd", [L, 256, 128])
    w_in_d = din("w_in", [L, D, 2048])
    w_o_d = din("w_o", [L, D, D])
    cf_pw_d = din("cf_pw", [L, 256, 256])
    rw_d = din("router_w", [D, NE])
    wg_d = din("exp_w_gate", [L, NE, D, DE])
    wu_d = din("exp_w_up", [L, NE, D, DE])
    wd_d = din("exp_w_down", [L, NE, DE, D])
    out_d = nc.dram_tensor("out", [TM, D], F32, kind="ExternalOutput").ap()

    with ExitStack() as st:
        def sb(name, F, dt=F32, P=128):
            h = st.enter_context(nc.sbuf_tensor("s_" + name, [P, F], dt))
            return Buf(h, name, P, F, dt)

        def ps(name, F, dt=F32):
            h = st.enter_context(nc.psum_tensor(name, [128, F], dt))
            bf = Buf(h, name, 128, F, dt)
            bf.whole = True
            return bf

        semnames = ["pe", "act", "dve", "pool", "sp", "dx", "dc", "dpar", "dwin", "dwo",
                    "dwg0", "dwu0", "dwd0", "dwg1", "dwu1", "dwd1", "dout", "dln2", "dcp", "dparp"]
        sems = {k: st.enter_context(nc.semaphore(k)) for k in semnames}
        em = Emitter(nc, sems)
        PE, ACT, DVE, POOL, SP = em.pe, em.act, em.dve, em.pool, em.sp

        xtok = sb("xtok", NT * D)
        xT = sb("xT", XT_COLS, BF16)
        wbuf = sb("wbuf", 24576, BF16)
        lnp = sb("lnp", 2048)
        bc3 = sb("bc3", 768)
        ppb = sb("ppb", NPP)
        identf = sb("identf", 128)
        identb = sb("identb", 128, BF16)
        maskT = sb("maskT", 128)
        ones = sb("ones", 128)
        rcfix = sb("rcfix", 32)
        invw = sb("invw", 2)
        hflag = sb("hflag", 1)
        rbb = sb("rbb", NE)
        wmT = sb("wmT", 512, BF16)
        pwb = sb("pwb", 512, BF16)
        pbd = sb("pbd", 256, BF16)
        rwb = sb("rwb", 8 * NE, BF16)
        gtmp = sb("gtmp", 512)

        def gates(a=0, c=NT * NE):
            return gtmp(a, c)
        yT = sb("yT", 8 * NB, BF16)
        SW = NB + 32
        NS = 9
        ZL0, CH0, TM0 = 0, 2, 4
        scr = sb("scr", NS * SW)
        scrb = sb("scrb", 2 * 4 * XNB, BF16)
        HBW = 30 + NB + 2
        hbb = sb("hbb", 2 * HBW, BF16)
        dg = sb("dg", 4 * 128, BF16)
        sm = sb("sm", 384)

        def rt(a, c):
            return scr(2 * SW + a, 2 * SW + c)

        def SG(k, nb):
            return scr(k * SW, k * SW + nb)

        xbs = [(lambda a=0, c=D, k=k: scrb(2048 + k * D + a, 2048 + k * D + c)) for k in range(2)]

        def wmS(a=0, c=512):
            return scr(7 * SW + a, 7 * SW + c)

        ACC = [lambda a, c, p0=0, p1=128, k=k: scr((7 + k) * SW + a, (7 + k) * SW + c, p0, p1) for k in range(2)]

        def S(i, f0=0, f1=SW, p0=0, p1=128):
            return scr(i * SW + f0, i * SW + f1, p0, p1)

        B = [ps("pb%d" % i, 512) for i in range(6)]
        pTs = [ps("pT%d" % i, 1024, BF16) for i in range(2)]

        def mm(out, lhsT, rhs, start, stop):
            em.op(PE, lambda e: e.matmul(out.ap, lhsT.ap, rhs.ap, start=start, stop=stop), w=[out], r=[lhsT, rhs])

        def act(out, in_, func, bias=None, scale=None, extra_r=()):
            kw = {}
            if bias is not None:
                kw["bias"] = bias.ap if isinstance(bias, Ref) else bias
            if scale is not None:
                kw["scale"] = scale.ap if isinstance(scale, Ref) else scale
            rr = [in_] + [a for a in (bias, scale) if isinstance(a, Ref)] + list(extra_r)
            em.op(ACT, lambda e: e.activation(out=out.ap, in_=in_.ap, func=func, **kw), w=[out], r=rr)

        def tt(E, out, a, b, op):
            em.op(E, lambda e: e.tensor_tensor(out=out.ap, in0=a.ap, in1=b.ap, op=op), w=[out], r=[a, b])

        def ts(E, out, a, s1, op0, s2=None, op1=None):
            rr = [a] + [s for s in (s1, s2) if isinstance(s, Ref)]
            a1 = s1.ap if isinstance(s1, Ref) else s1
            a2 = s2.ap if isinstance(s2, Ref) else s2
            if op1 is None:
                em.op(E, lambda e: e.tensor_scalar(out=out.ap, in0=a.ap, scalar1=a1, scalar2=None, op0=op0), w=[out], r=rr)
            else:
                em.op(E, lambda e: e.tensor_scalar(out=out.ap, in0=a.ap, scalar1=a1, scalar2=a2, op0=op0, op1=op1),
                      w=[out], r=rr)

        def stt(E, out, a, s, b, op0, op1):
            rr = [a, b] + ([s] if isinstance(s, Ref) else [])
            sv = s.ap if isinstance(s, Ref) else s
            em.op(E, lambda e: e.scalar_tensor_tensor(out=out.ap, in0=a.ap, scalar=sv, in1=b.ap, op0=op0, op1=op1),
                  w=[out], r=rr)

        def cpy(E, out, in_):
            if E is ACT:
                em.op(E, lambda e: e.copy(out=out.ap, in_=in_.ap), w=[out], r=[in_])
            else:
                em.op(E, lambda e: e.tensor_copy(out=out.ap, in_=in_.ap), w=[out], r=[in_])

        def powm(out, in_, n):
            act(out, in_, AF.Ln)
            act(out, out, AF.Exp, scale=-0.5)

        def xloc(t0):
            if t0 < 128:
                return 0, t0
            return 1 + (t0 - 128) // XNB, (t0 - 128) % XNB

        def xT_tok(t0, n, kc):
            xb_, c0 = xloc(t0)
            nbx = XBLOCKS[xb_][1]
            assert c0 + n <= nbx
            return xT(XOFF[xb_] + kc * nbx + c0, XOFF[xb_] + kc * nbx + c0 + n)

        def xtile(i, f0=0, f1=D):
            return xtok(i * D + f0, i * D + f1)

        def pipeline(items, stages):
            n, m = len(items), len(stages)
            for step in range(n + m - 1):
                for k in range(m - 1, -1, -1):
                    idx = step - k
                    if 0 <= idx < n:
                        stages[k](idx, items[idx])

        def SMT(i, a, c):
            return sm(i * 20 + a, i * 20 + c)

        def ln_stages(goff, ring0=0):
            def sA(r, i):
                for c in range(2):
                    em.op(DVE, lambda e, c=c: e.bn_stats(out=SMT(i, c * 6, c * 6 + 6).ap, in_=xtile(i, c * 512, (c + 1) * 512).ap),
                          w=[SMT(i, c * 6, c * 6 + 6)], r=[xtile(i, c * 512, (c + 1) * 512)])
                em.op(DVE, lambda e: e.bn_aggr(out=SMT(i, 12, 14).ap, in_=SMT(i, 0, 12).ap), w=[SMT(i, 12, 14)],
                      r=[SMT(i, 0, 12)])
                ts(DVE, SMT(i, 14, 15), SMT(i, 13, 14), LN_EPS, ALU.add)

            def sB(r, i):
                powm(SMT(i, 15, 16), SMT(i, 14, 15), 1)
                stt(DVE, SMT(i, 16, 17), SMT(i, 12, 13), -1.0, SMT(i, 15, 16), ALU.mult, ALU.mult)

            def sC(r, i):
                act(xtile(i), xtile(i), AF.Identity, bias=SMT(i, 16, 17), scale=SMT(i, 15, 16))

            def sD(r, i):
                tt(DVE, xtile(i), xtile(i), lnp(goff, goff + D), ALU.mult)
                tt(POOL, xtile(i), xtile(i), lnp(goff + D, goff + 2 * D), ALU.add)
            return [sA, sB, sC, sD]

        def xT_stages(scale0=None, alpha_after=False, pt_fixed=None):
            def sE(r, i):
                xb = xbs[r % 2]
                if scale0 is not None and i == 0:
                    act(xb(), xtile(i), AF.Identity, scale=scale0)
                else:
                    cpy(ACT, xb(), xtile(i))
                if alpha_after:
                    em.op(ACT, lambda e: e.mul(out=xtile(i).ap, in_=xtile(i).ap, mul=ALPHA), w=[xtile(i)], r=[xtile(i)])

            def sF(r, i):
                xb = xbs[r % 2]
                pT = pTs[r % 2 if pt_fixed is None else pt_fixed]
                for kc in range(8):
                    em.op(PE, lambda e, kc=kc: e.transpose(out=pT(kc * 128, (kc + 1) * 128).ap, in_=xb(kc * 128, (kc + 1) * 128).ap,
                                                    identity=identb().ap),
                          w=[pT(kc * 128, (kc + 1) * 128)], r=[xb(kc * 128, (kc + 1) * 128), identb()])

            def sG(r, i):
                pT = pTs[r % 2 if pt_fixed is None else pt_fixed]
                b, c0 = xloc(i * 128)
                nb = XBLOCKS[b][1]
                dst = xT(XOFF[b], XOFF[b] + 8 * nb).v(
                    lambda ap: ap.rearrange("p (k t) -> p k t", k=8)[:, :, c0:c0 + 128])
                src = pT().v(lambda ap: ap.rearrange("p (k t) -> p k t", k=8))
                cpy(ACT, dst, src)
            return [sE, sF, sG]

        def rms_group(yf, nb, gcol, c0):
            sq = S(TM0 + 2, 0, nb)
            for j in range(2):
                act(sq, yf[j], AF.Square)
                mm(B[4](0, nb), ones(), sq, j == 0, j == 1)
            ve = S(TM0 + 1, 0, nb)
            ts(DVE, ve, B[4](0, nb), 1.0 / 256.0, ALU.mult, RMS_EPS, ALU.add)
            powm(ve, ve, nb)
            for j in range(2):
                stt(DVE, yT((c0 + j) * nb, (c0 + j + 1) * nb), yf[j], ppb(gcol + j, gcol + j + 1), ve, ALU.mult, ALU.mult)

        for i in range(NT):
            em.dma(SP, xtile(i).ap, x_d[i * 128:(i + 1) * 128, :], "dx", w=[xtile(i)], group_left=NT - 1 - i)
        cl = [(identf, ident_d), (maskT, maskT_d), (rcfix, rcfix_d), (invw, invw_d), (hflag, hflag_d), (rbb, rbb_d)]
        for n, (bf, d) in enumerate(cl):
            em.dma(SP, bf().ap, d, "dc", w=[bf()], group_left=len(cl) - 1 - n)
        em.dma(POOL, rwb().ap.rearrange("p (k n) -> p k n", k=8), rw_d.rearrange("(k p) n -> p k n", p=128), "dcp",
               w=[rwb()])
        cpy(ACT, identb(), identf())
        em.op(DVE, lambda e: e.memset(ones().ap, 1.0), w=[ones()])

        def load_layer_weights(l):
            for q in range(4):
                em.dma(POOL, wbuf(q * 4096, (q + 1) * 4096).ap.rearrange("p (k n) -> p k n", k=2),
                       w_in_d[l, q * 256:(q + 1) * 256, :].rearrange("(k p) n -> p k n", p=128), "dwin",
                       w=[wbuf(q * 4096, (q + 1) * 4096)], group_left=3 - q)
            for q in range(2):
                em.dma(POOL, wbuf(16384 + q * 4096, 16384 + (q + 1) * 4096).ap.rearrange("p (k n) -> p k n", k=4),
                       w_o_d[l, q * 512:(q + 1) * 512, :].rearrange("(k p) n -> p k n", p=128), "dwo",
                       w=[wbuf(16384 + q * 4096, 16384 + (q + 1) * 4096)], group_left=1 - q)

        def load_layer_params(l):
            lst = [(SP, ppb(), pp_d[l]), (SP, bc3(), bc3_d[l]), (SP, lnp(), lnp_d[l, :, 0:2048]), (SP, wmS(), gmwT_d[l])]
            n = len(lst)
            for k, (E, rf, d) in enumerate(lst):
                em.dma(E, rf.ap, d, "dpar", w=[rf], group_left=n - 1 - k)
            em.dma(POOL, pwb().ap.rearrange("p (k n) -> p k n", k=2),
                   cf_pw_d[l].rearrange("(k p) n -> p k n", p=128), "dparp", w=[pwb()], group_left=1)
            em.dma(POOL, pbd().ap.rearrange("p (k n) -> p k n", k=2),
                   pbd_d[l].rearrange("(k p) n -> p k n", p=128), "dparp", w=[pbd()], group_left=0)
            tt(DVE, wmT().v(lambda ap: ap.rearrange("p (h t) -> p h t", h=4)),
               wmS().v(lambda ap: ap.rearrange("p (h t) -> p h t", h=4)),
               maskT().v(lambda ap: ap.rearrange("p (o t) -> p o t", o=1).to_broadcast([128, 4, 128])), ALU.mult)

        def load_expert(l, e):
            s = e % 2
            base = s * 12288
            em.dma(POOL, wbuf(base, base + 4096).ap.rearrange("p (k n) -> p k n", k=8),
                   wg_d[l, e].rearrange("(k p) n -> p k n", p=128), "dwg%d" % s, w=[wbuf(base, base + 4096)])
            em.dma(POOL, wbuf(base + 4096, base + 8192).ap.rearrange("p (k n) -> p k n", k=8),
                   wu_d[l, e].rearrange("(k p) n -> p k n", p=128), "dwu%d" % s, w=[wbuf(base + 4096, base + 8192)])
            em.dma(POOL, wbuf(base + 8192, base + 12288).ap.rearrange("p (k n) -> p k n", k=4),
                   wd_d[l, e].rearrange("(k p) n -> p k n", p=128), "dwd%d" % s, w=[wbuf(base + 8192, base + 12288)])

        load_layer_weights(0)
        load_layer_params(0)
        pipeline(list(range(NT)), xT_stages())

        def win(kc, c0, c1):
            return wbuf(kc * 2048 + c0, kc * 2048 + c1)

        def wo(kc, c0, c1):
            return wbuf(16384 + kc * 1024 + c0, 16384 + kc * 1024 + c1)

        zrot = [0]
        dgi = [0]
        pre_e0 = [False]

        def zbank():
            b = B[2 + zrot[0] % 2]
            zrot[0] += 1
            return b

        def zmm(bank, t0, nb, col0):
            for kc in range(8):
                mm(bank(0, nb), win(kc, col0, col0 + 128), xT_tok(t0, nb, kc), kc == 0, kc == 7)

        def finish_dump():
            for i in range(1, NT):
                em.dma(SP, out_d[(i - 1) * 128:i * 128, :], xtile(i).ap, "dout", r=[xtile(i)])
            SP.e.wait_ge(sems["dout"], em.dma_count["dout"])
            build.stats = (em.n_inst, em.n_wait)

        if stop == "setup":
            finish_dump()
            return nc
        for l in range(n_layers):
            for j in range(2):
                em.op(DVE, lambda e: e.memset(hbb(j * HBW, j * HBW + 30).ap, 0.0), w=[hbb(j * HBW, j * HBW + 30)])
                em.op(DVE, lambda e: e.memset(S(ZL0 + j, 0, 15).ap, 0.0), w=[S(ZL0 + j, 0, 15)])
                em.op(DVE, lambda e: e.memset(S(CH0 + j, 0, 2).ap, 0.0), w=[S(CH0 + j, 0, 2)])

            last = (l == n_layers - 1)
            def G_section(b):
                t0, nb = MBLOCKS[b]
                tiles = list(range(t0 // 128, (t0 + nb) // 128))
                state_only = last and b == 0 and stop is None
                GS = 340

                def g_bufs(r):
                    p = r % 2
                    p = 0
                    pUV, pS_ = B[0], B[1]
                    sl = [(lambda a, c: gtmp(a, c)), (lambda a, c: gtmp(256 + a, 256 + c)), (lambda a, c: gtmp(a, c))]
                    u_sb, vn, yg = sl[0](0, 256), sl[1](0, 256), sl[2](0, 256)
                    q = lambda a, c: sm(GS + a, GS + c)
                    vb = lambda a, c: scrb(1024 + a, 1024 + c)
                    return pUV, pS_, sl, u_sb, vn, yg, q, vb, pTs[1]

                def g0(r, i):
                    pUV = g_bufs(r)[0]
                    for kc in range(8):
                        mm(pUV(), xT_tok(i * 128, 128, kc), win(kc, 0, 512), kc == 0, kc == 7)

                def g1(r, i):
                    pUV, pS_, sl, u_sb, vn, yg, q, vb, pT = g_bufs(r)
                    cpy(ACT, u_sb, pUV(0, 256))
                    cpy(ACT, vn, pUV(256, 512))

                def g2(r, i):
                    pUV, pS_, sl, u_sb, vn, yg, q, vb, pT = g_bufs(r)
                    em.op(DVE, lambda e: e.bn_stats(out=q(0, 6).ap, in_=vn.ap), w=[q(0, 6)], r=[vn])
                    em.op(DVE, lambda e: e.bn_aggr(out=q(6, 8).ap, in_=q(0, 6).ap), w=[q(6, 8)], r=[q(0, 6)])
                    ts(DVE, q(8, 9), q(7, 8), LN_EPS, ALU.add)

                def g3(r, i):
                    pUV, pS_, sl, u_sb, vn, yg, q, vb, pT = g_bufs(r)
                    powm(q(9, 10), q(8, 9), 1)

                def g4(r, i):
                    pUV, pS_, sl, u_sb, vn, yg, q, vb, pT = g_bufs(r)
                    ts(DVE, vn, vn, q(6, 7), ALU.subtract, q(9, 10), ALU.mult)
                    tt(DVE, vn, vn, bc3(0, 256), ALU.mult)
                    tt(DVE, vb(0, 256), vn, bc3(256, 512), ALU.add)

                def g5(r, i):
                    pUV, pS_, sl, u_sb, vn, yg, q, vb, pT = g_bufs(r)
                    for h in range(4):
                        mm(pS_(64 * h, 64 * h + 64), wmT(128 * h, 128 * h + 128), vb(64 * h, 64 * h + 64), True, True)

                def g6(r, i):
                    pUV, pS_, sl, u_sb, vn, yg, q, vb, pT = g_bufs(r)
                    for h in range(4):
                        stt(DVE, sl[2](64 * h, 64 * h + 64), pS_(64 * h, 64 * h + 64), ppb(PP_BS + h, PP_BS + h + 1),
                            sl[0](64 * h, 64 * h + 64), ALU.add, ALU.mult)
                    em.op(DVE, lambda e: e.bn_stats(out=q(10, 16).ap, in_=yg.ap), w=[q(10, 16)], r=[yg])
                    em.op(DVE, lambda e: e.bn_aggr(out=q(16, 18).ap, in_=q(10, 16).ap), w=[q(16, 18)], r=[q(10, 16)])
                    stt(DVE, q(18, 19), q(16, 17), q(16, 17), q(17, 18), ALU.mult, ALU.add)
                    ts(DVE, q(18, 19), q(18, 19), RMS_EPS, ALU.add)

                def g7(r, i):
                    pUV, pS_, sl, u_sb, vn, yg, q, vb, pT = g_bufs(r)
                    powm(q(19, 20), q(18, 19), 1)
                    stt(DVE, vb(256, 512), yg, q(19, 20), bc3(512, 768), ALU.mult, ALU.mult)

                def g8(r, i):
                    pUV, pS_, sl, u_sb, vn, yg, q, vb, pT = g_bufs(r)
                    for c in range(2):
                        em.op(PE, lambda e, c=c: e.transpose(out=pT(c * 128, (c + 1) * 128).ap,
                                                        in_=vb(256 + c * 128, 256 + (c + 1) * 128).ap,
                                                        identity=identb().ap),
                              w=[pT(c * 128, (c + 1) * 128)], r=[vb(256 + c * 128, 256 + (c + 1) * 128), identb()])

                def g9(r, i):
                    pT = g_bufs(r)[8]
                    c0 = i * 128 - t0
                    dst = yT(0, 2 * nb).v(lambda ap: ap.rearrange("p (k t) -> p k t", k=2)[:, :, c0:c0 + 128])
                    cpy(ACT, dst, pT(0, 256).v(lambda ap: ap.rearrange("p (k t) -> p k t", k=2)))

                for i_ in tiles:
                    for gs in [g0, g1, g2, g3, g4, g5, g6, g7, g8, g9]:
                        gs(0, i_)


            def CPS_section(b):
                t0, nb = MBLOCKS[b]
                tiles = list(range(t0 // 128, (t0 + nb) // 128))
                state_only = last and b == 0 and stop is None
                accs = []
                for j in range(2):
                    pa = zbank()
                    zmm(pa, t0, nb, 512 + j * 128)
                    pg = zbank()
                    zmm(pg, t0, nb, 768 + j * 128)
                    sig = S(TM0 + j, 0, nb)
                    act(sig, pg(0, nb), AF.Tanh, scale=0.5)
                    hb = lambda a, c, j=j: hbb(j * HBW + a, j * HBW + c)
                    stt(DVE, hb(30, 30 + nb), sig, 1.0, pa(0, nb), ALU.add, ALU.mult)
                hbs = [(lambda a, c, j=j: hbb(j * HBW + a, j * HBW + c)) for j in range(2)]
                if state_only:
                    for j in range(2):
                        cpy(ACT, hbs[j](0, 30), hbs[j](nb, nb + 30))
                else:
                    pcs = [zbank(), zbank()]
                    for k in range(31):
                        for j in range(2):
                            slot = dgi[0] % 4
                            dgi[0] += 1
                            wcol = ppb(PP_DW + j * 31 + k, PP_DW + j * 31 + k + 1)
                            GE = (POOL, ACT, POOL, ACT)[slot]
                            if GE is ACT:
                                act(dg(slot * 128, (slot + 1) * 128), identf(), AF.Identity, scale=wcol)
                            elif GE is DVE:
                                ts(GE, dg(slot * 128, (slot + 1) * 128), identf(), wcol, ALU.mult)
                            else:
                                ts(GE, dg(slot * 128, (slot + 1) * 128), identf(), wcol, ALU.mult, 0.0, ALU.add)
                            mm(pcs[j](0, nb), dg(slot * 128, (slot + 1) * 128), hbs[j](k, k + nb), k == 0, k == 30)
                    for j in range(2):
                        act(ACC[j](0, nb), pcs[j](0, nb), AF.Identity, bias=ppb(PP_DWB + j, PP_DWB + j + 1), scale=0.5)
                    for j in range(2):
                        cpy(ACT, hbs[j](0, 30), hbs[j](nb, nb + 30))
                        accs.append(ACC[j](0, nb))
                if state_only:
                    for j in range(2):
                        pz = zbank()
                        zmm(pz, t0, nb, 1024 + j * 128)
                        cpy(ACT, S(ZL0 + j, 15, 15 + nb), pz(0, nb))
                        cpy(ACT, S(ZL0 + j, 0, 15), S(ZL0 + j, nb, nb + 15))
                        pC_ = zbank()
                        zmm(pC_, t0, nb, 1536 + j * 128)
                        pH_ = zbank()
                        zmm(pH_, t0, nb, 1792 + j * 128)
                        cs = S(TM0, 0, nb)
                        cpy(ACT, cs, pC_(0, nb))
                        tt(DVE, S(CH0 + j, 2, 2 + nb), pH_(0, nb), cs, ALU.mult)
                        cpy(ACT, S(CH0 + j, 0, 2), S(CH0 + j, nb, nb + 2))
                    return
                sq = S(TM0 + 2, 0, nb)
                for j in range(2):
                    act(sq, accs[j], AF.Square)
                    mm(B[5](0, nb), ones(), accs[j], j == 0, j == 1)
                    mm(B[4](0, nb), ones(), sq, j == 0, j == 1)
                mean = S(TM0, 0, nb)
                ts(DVE, mean, B[5](0, nb), 1.0 / 256.0, ALU.mult)
                msq = S(TM0 + 1, 0, nb)
                tt(DVE, msq, mean, mean, ALU.mult)
                ve = S(TM0 + 2, 0, nb)
                ts(DVE, ve, B[4](0, nb), 1.0 / 256.0, ALU.mult, LN_EPS, ALU.add)
                tt(DVE, ve, ve, msq, ALU.subtract)
                powm(ve, ve, nb)
                for j in range(2):
                    tt(DVE, accs[j], accs[j], mean, ALU.subtract)
                    tt(DVE, accs[j], accs[j], ve, ALU.mult)
                    act(scrb(j * nb, (j + 1) * nb), accs[j], AF.Silu, bias=ppb(PP_CLB + j, PP_CLB + j + 1),
                        scale=ppb(PP_CLG + j, PP_CLG + j + 1))
                yf = []
                for jo in range(2):
                    for j in range(2):
                        mm(B[5](0, nb), pwb(j * 256 + jo * 128, j * 256 + (jo + 1) * 128), scrb(j * nb, (j + 1) * nb),
                           j == 0, j == 1)
                    cpy(ACT, ACC[jo](0, nb), B[5](0, nb))
                    yf.append(ACC[jo](0, nb))
                rms_group(yf, nb, PP_MG + 0, 2)

                zls = [(lambda a, c, p0=0, p1=128, j=j: S(ZL0 + j, a, c, p0, p1)) for j in range(2)]
                Xs = [(lambda a, c, p0=0, p1=128: S(TM0, a, c, p0, p1)), (lambda a, c, p0=0, p1=128: S(TM0 + 2, a, c, p0, p1))]
                Ys = [(lambda a, c, p0=0, p1=128: S(TM0 + 1, a, c, p0, p1)), ACC[1]]
                pls = [(lambda a, c, p0=0, p1=128, j=j: scrb(j * nb + a, j * nb + c, p0, p1)) for j in range(2)]
                pzb = [B[2], B[3]]
                pob = [B[5], B[4]]
                for j in range(2):
                    zmm(pzb[j], t0, nb, 1024 + j * 128)
                for j in range(2):
                    cpy(ACT, zls[j](15, 15 + nb), pzb[j](0, nb))
                for j in range(2):
                    tt(DVE, Xs[j](1, 15 + nb), zls[j](1, 15 + nb), zls[j](0, 14 + nb), ALU.add)
                for j in range(2):
                    tt(DVE, Ys[j](3, 15 + nb), Xs[j](3, 15 + nb), Xs[j](1, 13 + nb), ALU.add)
                tt(DVE, Xs[1](7, 15 + nb), Ys[1](7, 15 + nb), Ys[1](3, 11 + nb), ALU.add)
                tt(DVE, Ys[1](15, 15 + nb), Xs[1](15, 15 + nb), Xs[1](7, 7 + nb), ALU.add)
                for (p0, p1) in ((0, 64), (64, 128)):
                    for j in range(2):
                        src = Xs[j] if p0 == 0 else Ys[j]
                        stt(DVE, pls[j](0, nb, p0, p1), src(15, 15 + nb, p0, p1), invw(j, j + 1, p0, p1),
                            zls[j](15, 15 + nb, p0, p1), ALU.mult, ALU.subtract)
                if b == 1:
                    for j in range(2):
                        for (p0, p1) in ((0, 64), (64, 128)):
                            src = Xs[j] if p0 == 0 else Ys[j]
                            tmp = sm(368, 384, p0, p1)
                            tt(DVE, tmp, src(15, 31, p0, p1), rcfix(j * 16, j * 16 + 16, p0, p1), ALU.mult)
                            tt(DVE, pls[j](0, 16, p0, p1), tmp, zls[j](15, 31, p0, p1), ALU.subtract)
                for j in range(2):
                    cpy(ACT, zls[j](0, 15), zls[j](nb, nb + 15))
                for j in range(2):
                    mm(pob[j](0, nb), pbd(j * 128, (j + 1) * 128), pls[j](0, nb), True, True)
                for j in range(2):
                    act(ACC[j](0, nb), pob[j](0, nb), AF.Identity, scale=ppb(PP_PSC + j, PP_PSC + j + 1))
                rms_group([ACC[0](0, nb), ACC[1](0, nb)], nb, PP_MG + 2, 4)

                def zmm_to(bank, col0):
                    zmm(bank, t0, nb, col0)
                    return bank
                sbank = [(B[2], B[3], B[2]), (B[4], B[5], B[4])]
                chls = [(lambda a, c, j=j: S(CH0 + j, a, c)) for j in range(2)]
                css = [S(TM0 + j, 0, nb) for j in range(2)]
                for j in range(2):
                    zmm_to(sbank[j][0], 1536 + j * 128)
                    zmm_to(sbank[j][1], 1792 + j * 128)
                for j in range(2):
                    cpy(ACT, css[j], sbank[j][0](0, nb))
                for j in range(2):
                    tt(DVE, chls[j](2, 2 + nb), sbank[j][1](0, nb), css[j], ALU.mult)
                for j in range(2):
                    zmm_to(sbank[j][2], 1280 + j * 128)
                for j in range(2):
                    ts(DVE, ACC[j](0, nb), chls[j](0, nb), ppb(PP_SCW + j * 3, PP_SCW + j * 3 + 1), ALU.mult)
                for k in (1, 2):
                    for j in range(2):
                        stt(DVE, ACC[j](0, nb), chls[j](k, k + nb), ppb(PP_SCW + j * 3 + k, PP_SCW + j * 3 + k + 1),
                            ACC[j](0, nb), ALU.mult, ALU.add)
                for j in range(2):
                    tt(DVE, ACC[j](0, nb), sbank[j][2](0, nb), ACC[j](0, nb), ALU.mult)
                for j in range(2):
                    cpy(ACT, chls[j](0, 2), chls[j](nb, nb + 2))
                rms_group([ACC[0](0, nb), ACC[1](0, nb)], nb, PP_MG + 4, 6)


            def O0_section(b):
                t0, nb = MBLOCKS[b]
                tiles = list(range(t0 // 128, (t0 + nb) // 128))
                for r, i in enumerate(tiles):
                    c0 = i * 128 - t0
                    banks = (B[0], B[1]) if r % 2 == 0 else (B[2], B[3])
                    for nh in range(2):
                        for kc in range(8):
                            mm(banks[nh](), yT(kc * nb + c0, kc * nb + c0 + 128), wo(kc, nh * 512, (nh + 1) * 512),
                               kc == 0, kc == 7)
                        stt(DVE, xtile(i, nh * 512, (nh + 1) * 512), xtile(i, nh * 512, (nh + 1) * 512), ALPHA, banks[nh](),
                            ALU.mult, ALU.add)

            def A_section(b):
                t0, nb = MBLOCKS[b]
                tiles = list(range(t0 // 128, (t0 + nb) // 128))
                pipeline(tiles, ln_stages(0) + xT_stages(alpha_after=True, pt_fixed=0))

            def recorded(fn, *a):
                em.rec = []
                fn(*a)
                r_, em.rec = em.rec, None
                return r_

            def interleave(ra, rb):
                na, nb_ = len(ra), len(rb)
                out, ia, ib = [], 0, 0
                while ia < na or ib < nb_:
                    if ib >= nb_ or (ia < na and ia * nb_ <= ib * na):
                        out.append(ra[ia]); ia += 1
                    else:
                        out.append(rb[ib]); ib += 1
                return out

            nblk = len(MBLOCKS)
            skip0 = last and stop is None
            if skip0:
                CPS_section(0)
            fb = 1 if skip0 else 0
            G_section(fb)
            CPS_section(fb)
            O0_section(fb)
            for b in range(fb, nblk):
                ra = recorded(A_section, b)
                if b + 1 < nblk:
                    x = interleave(ra, recorded(G_section, b + 1))
                    for rec in interleave(x, recorded(CPS_section, b + 1)):
                        em.replay(rec)
                    O0_section(b + 1)
                    pass
                else:
                    for rec in ra:
                        em.replay(rec)
            if stop == "mixer":
                finish_dump()
                return nc
            em.dma(SP, lnp().ap, lnp_d[l, :, 2048:4096], "dln2", w=[lnp()])
            if not pre_e0[0]:
                load_expert(l, 0)
            pre_e0[0] = False
            load_expert(l, 1)
            NL = NT * NE
            for i in range(NT):
                for kc in range(8):
                    mm(B[4](i * NE, (i + 1) * NE), xT_tok(i * 128, 128, kc), rwb(kc * NE, (kc + 1) * NE), kc == 0, kc == 7)
            lg = rt(0, NL)
            sel = rt(NL, 2 * NL)
            msk = rt(2 * NL, 3 * NL)
            eq1 = rt(3 * NL, 4 * NL)
            eq2 = rt(4 * NL, 5 * NL)
            tmp = rt(5 * NL, 6 * NL)
            pr = rt(3 * NL, 3 * NL + 68 * 6)
            o = 6 * NL
            gs = rt(o, o + 68)
            gmax = rt(o + 68, o + 85)
            m1 = rt(o + 85, o + 102)
            la = rt(o + 102, o + 119)
            lb = rt(o + 119, o + 136)
            g1 = rt(o + 136, o + 153)
            g2 = rt(o + 153, o + 170)
            v4 = lambda rf: rf.v(lambda ap: ap.rearrange("p (g f) -> p g f", f=4))
            v16 = lambda rf: rf.v(lambda ap: ap.rearrange("p (t e) -> p t e", e=NE))
            bc16 = lambda rf: rf.v(lambda ap: ap.rearrange("p (t o) -> p t o", o=1).to_broadcast([128, NT, NE]))
            cpy(DVE, lg, B[4](0, NL))
            tt(DVE, v16(sel), v16(lg),
               rbb().v(lambda ap: ap.rearrange("p (o e) -> p o e", o=1).to_broadcast([128, NT, NE])), ALU.add)
            s4v = sel.v(lambda ap: ap.rearrange("p (g f) -> p g f", f=4))
            p6 = lambda a, c: pr.v(lambda ap: ap.rearrange("p (g f) -> p g f", f=6)[:, :, a:c])
            s4s = lambda a, c: sel.v(lambda ap: ap.rearrange("p (g f) -> p g f", f=4)[:, :, a:c])
            tt(DVE, p6(0, 3), s4s(0, 3), s4s(1, 4), ALU.add)
            tt(DVE, p6(3, 5), s4s(0, 2), s4s(2, 4), ALU.add)
            tt(DVE, p6(5, 6), s4s(0, 1), s4s(3, 4), ALU.add)
            em.op(DVE, lambda e: e.reduce_max(out=gs.ap, in_=pr.ap.rearrange("p (g f) -> p g f", f=6), axis=AX.X),
                  w=[gs], r=[pr])
            em.op(DVE, lambda e: e.reduce_max(out=gmax.ap, in_=gs.ap.rearrange("p (t g) -> p t g", g=4), axis=AX.X),
                  w=[gmax], r=[gs])
            ing = rt(o + 170, o + 238)
            tt(DVE, ing.v(lambda ap: ap.rearrange("p (t g) -> p t g", g=4)),
               gs.v(lambda ap: ap.rearrange("p (t g) -> p t g", g=4)),
               gmax.v(lambda ap: ap.rearrange("p (t o) -> p t o", o=1).to_broadcast([128, NT, 4])), ALU.is_equal)
            ts(DVE, ing, ing, 1.0, ALU.subtract, 1e30, ALU.mult)
            tt(DVE, v4(msk), v4(sel),
               ing.v(lambda ap: ap.rearrange("p (g o) -> p g o", o=1).to_broadcast([128, 68, 4])), ALU.add)
            em.op(DVE, lambda e: e.reduce_max(out=m1.ap, in_=msk.ap.rearrange("p (t e) -> p t e", e=NE), axis=AX.X),
                  w=[m1], r=[msk])
            tt(DVE, v16(eq1), v16(msk), bc16(m1), ALU.is_equal)
            stt(DVE, msk, eq1, -1e30, msk, ALU.mult, ALU.add)
            em.op(DVE, lambda e: e.reduce_max(out=m1.ap, in_=msk.ap.rearrange("p (t e) -> p t e", e=NE), axis=AX.X),
                  w=[m1], r=[msk])
            tt(DVE, v16(eq2), v16(msk), bc16(m1), ALU.is_equal)
            tt(DVE, tmp, eq1, lg, ALU.mult)
            em.op(DVE, lambda e: e.reduce_sum(out=la.ap, in_=tmp.ap.rearrange("p (t e) -> p t e", e=NE), axis=AX.X),
                  w=[la], r=[tmp])
            tt(DVE, tmp, eq2, lg, ALU.mult)
            em.op(DVE, lambda e: e.reduce_sum(out=lb.ap, in_=tmp.ap.rearrange("p (t e) -> p t e", e=NE), axis=AX.X),
                  w=[lb], r=[tmp])
            tt(DVE, la, la, lb, ALU.subtract)
            act(g1, la, AF.Sigmoid)
            act(g2, la, AF.Sigmoid, scale=-1.0)
            tt(DVE, v16(eq1), v16(eq1), bc16(g1), ALU.mult)
            tt(DVE, v16(eq2), v16(eq2), bc16(g2), ALU.mult)
            tt(DVE, gates(), eq1, eq2, ALU.add)
            if stop == "router":
                finish_dump()
                return nc

            def ln2_block(tl):
                if l + 1 < n_layers:
                    pipeline(tl, ln_stages(0) + xT_stages(scale0=hflag()))
                else:
                    def sOut(r, i):
                        if i >= 1:
                            em.dma(SP, out_d[(i - 1) * 128:i * 128, :], xtile(i).ap, "dout", r=[xtile(i)])
                    pipeline(tl, ln_stages(0) + [sOut])

            pend_ln = []
            ln2_st = ln_stages(0)

            def ln2_out(r, i):
                if i >= 1:
                    em.dma(SP, out_d[(i - 1) * 128:i * 128, :], xtile(i).ap, "dout", r=[xtile(i)])

            it = 0
            for e_ in range(NE):
                spread_ln = (e_ == NE - 1) and stop is None
                s = e_ % 2
                base = s * 12288
                for b, (t0, nb) in enumerate(XBLOCKS):
                    if last and b == 0 and stop is None:
                        continue
                    tiles = list(range(t0 // 128, (t0 + nb) // 128))
                    hb0 = (it % 2) * 4 * XNB
                    for fc in range(4):
                        pG = B[(fc % 2) * 2]
                        pU = B[(fc % 2) * 2 + 1]
                        for kc in range(8):
                            mm(pG(0, nb), wbuf(base + kc * 512 + fc * 128, base + kc * 512 + (fc + 1) * 128),
                               xT_tok(t0, nb, kc), kc == 0, kc == 7)
                        for kc in range(8):
                            mm(pU(0, nb), wbuf(base + 4096 + kc * 512 + fc * 128, base + 4096 + kc * 512 + (fc + 1) * 128),
                               xT_tok(t0, nb, kc), kc == 0, kc == 7)
                        sg = SG(fc % 2, nb)
                        act(sg, pG(0, nb), AF.Silu)
                        tt(DVE, scrb(hb0 + fc * nb, hb0 + (fc + 1) * nb), sg, pU(0, nb), ALU.mult)
                        if spread_ln and pend_ln:
                            for r_, i_ in enumerate(pend_ln[0]):
                                ln2_st[fc](r_, i_)
                    if spread_ln and pend_ln:
                        for r_, i_ in enumerate(pend_ln.pop()):
                            if last:
                                ln2_out(r_, i_)
                    for i in tiles:
                        c0 = i * 128 - t0
                        for nh in range(2):
                            pY = B[4 + nh]
                            for fc in range(4):
                                mm(pY(), scrb(hb0 + fc * nb + c0, hb0 + fc * nb + c0 + 128),
                                   wbuf(base + 8192 + fc * 1024 + nh * 512, base + 8192 + fc * 1024 + (nh + 1) * 512),
                                   fc == 0, fc == 3)
                            stt(DVE, xtile(i, nh * 512, (nh + 1) * 512), pY(), gates(i * NE + e_, i * NE + e_ + 1),
                                xtile(i, nh * 512, (nh + 1) * 512), ALU.mult, ALU.add)
                    it += 1
                    if spread_ln:
                        pend_ln.append(tiles)
                if stop == "e%d" % e_:
                    finish_dump()
                    return nc
                if e_ + 2 < NE:
                    load_expert(l, e_ + 2)
            if l + 1 < n_layers:
                load_layer_weights(l + 1)
            while pend_ln:
                tl = pend_ln.pop()
                if last:
                    ln2_block(tl)
                else:
                    pipeline(tl, ln_stages(0))
            if stop is None and not last:
                pipeline(list(range(NT)), xT_stages(scale0=hflag()))
            if stop is not None:
                ln2_block([i for i in range(NT)])
            if l + 1 < n_layers:
                load_layer_params(l + 1)

        SP.e.wait_ge(sems["dout"], em.dma_count["dout"])
        build.stats = (em.n_inst, em.n_wait)
    return nc


_CACHE = {}


def _host_consts(half):
    ident = np.eye(128, dtype=np.float32)
    s = np.arange(128)
    maskT = (s[:, None] <= s[None, :]).astype(np.float32)
    wins = np.array([2, 4, 8, 16])
    p = np.arange(128)
    rcfix = np.zeros((128, 32), np.float32)
    invw = np.zeros((128, 2), np.float32)
    for j in range(2):
        w = wins[2 * j + (p // 64)].astype(np.float32)
        invw[:, j] = 1.0 / w
        t = np.arange(16, dtype=np.float32)[None, :]
        if half == 0:
            rcfix[:, j * 16:(j + 1) * 16] = 1.0 / np.minimum(t + 1.0, w[:, None])
        else:
            rcfix[:, j * 16:(j + 1) * 16] = 1.0 / w[:, None]
    hflag = np.full((128, 1), float(half), np.float32)
    return ident, maskT, rcfix, invw, hflag


def kernel(x, w_in, gm_ln_g, gm_ln_b, gm_w_s, gm_b_s, cf_dw_w, cf_dw_b, cf_ln_g, cf_ln_b, cf_pw, pool_w,
           pool_scale, sc_w, mix_norm_g, w_o, ln1_g, ln1_b, router_w, router_b, exp_w_gate, exp_w_up,
           exp_w_down, ln2_g, ln2_b):
    f = lambda a: np.ascontiguousarray(np.asarray(a, dtype=np.float32))
    x = f(x)
    pp = np.zeros((L, 128, NPP), np.float32)
    bc3 = np.zeros((L, 128, 768), np.float32)
    lnp = np.zeros((L, 128, 4096), np.float32)
    gmwT = np.zeros((L, 128, 512), np.float32)
    pbd = np.zeros((L, 256, 128), np.float32)
    cf_dw_w, cf_dw_b, cf_ln_g, cf_ln_b = f(cf_dw_w), f(cf_dw_b), f(cf_ln_g), f(cf_ln_b)
    pool_scale, sc_w, mix_norm_g, gm_b_s = f(pool_scale), f(sc_w), f(mix_norm_g), f(gm_b_s)
    gm_w_s, pool_w = f(gm_w_s), f(pool_w)
    for l in range(L):
        for j in range(2):
            ch = slice(j * 128, (j + 1) * 128)
            pp[l, :, PP_DW + j * 31:PP_DW + (j + 1) * 31] = cf_dw_w[l][:, ch].T
            pp[l, :, PP_DWB + j] = cf_dw_b[l][ch]
            pp[l, :, PP_CLG + j] = cf_ln_g[l][ch]
            pp[l, :, PP_CLB + j] = cf_ln_b[l][ch]
            pp[l, :, PP_PSC + j] = pool_scale[l][ch]
            pp[l, :, PP_SCW + j * 3:PP_SCW + (j + 1) * 3] = sc_w[l][:, ch].T
        for c in range(6):
            pp[l, :, PP_MG + c] = mix_norm_g[l][256 + c * 128:256 + (c + 1) * 128]
        pp[l, :, PP_BS:PP_BS + 4] = gm_b_s[l].T
        bc3[l, :, 0:256] = f(gm_ln_g)[l][None, :]
        bc3[l, :, 256:512] = f(gm_ln_b)[l][None, :]
        bc3[l, :, 512:768] = mix_norm_g[l][None, 0:256]
        lnp[l, :, 0:1024] = f(ln1_g)[l][None, :]
        lnp[l, :, 1024:2048] = f(ln1_b)[l][None, :]
        lnp[l, :, 2048:3072] = f(ln2_g)[l][None, :]
        lnp[l, :, 3072:4096] = f(ln2_b)[l][None, :]
        for h in range(4):
            gmwT[l, :, h * 128:(h + 1) * 128] = gm_w_s[l, h].T
        for g in range(4):
            c, q = g // 2, g % 2
            pbd[l, c * 128 + q * 64:c * 128 + (q + 1) * 64, q * 64:(q + 1) * 64] = pool_w[l, g]
    rbb = np.ascontiguousarray(np.broadcast_to(f(router_b)[None, :], (128, NE))).astype(np.float32)
    shared = {
        "pp": pp, "bc3": bc3, "lnp": lnp, "gmwT": gmwT, "pbd": pbd, "rbb": rbb,
        "w_in": f(w_in), "w_o": f(w_o), "cf_pw": f(cf_pw), "router_w": f(router_w),
        "exp_w_gate": f(exp_w_gate), "exp_w_up": f(exp_w_up), "exp_w_down": f(exp_w_down),
    }
    in_maps = []
    for c in range(NCORES):
        bi, half = c // 2, c % 2
        xc = np.zeros((T, D), np.float32)
        xc[HALO:] = x[bi, half * TM:(half + 1) * TM]
        if half == 1:
            xc[:HALO] = x[bi, TM - HALO:TM]
        ident, maskT, rcfix, invw, hflag = _host_consts(half)
        m = dict(shared)
        m.update({"x": xc, "ident": ident, "maskT": maskT, "rcfix": rcfix, "invw": invw, "hflag": hflag})
        in_maps.append(m)
    if "nc" not in _CACHE:
        _CACHE["nc"] = build()
    res = run_bass_kernel_spmd(_CACHE["nc"], in_maps, core_ids=list(range(NCORES)))
    out = np.zeros((4, SEQ, D), np.float32)
    for c in range(NCORES):
        bi, half = c // 2, c % 2
        out[bi, half * TM:(half + 1) * TM] = res.results[c]["out"]
    return out
```

```python
from contextlib import ExitStack
import numpy as np
import concourse.bass as bass
import concourse.mybir as mybir
from concourse.bass_utils import run_bass_kernel_spmd

F32 = mybir.dt.float32
BF16 = mybir.dt.bfloat16
ALU = mybir.AluOpType
AF = mybir.ActivationFunctionType
AX = mybir.AxisListType
_DSZ = {F32: 4, BF16: 2}

L = 2
D = 1024
NCORES = 8
SEQ = 4096
TM = 2048
HALO = 128
T = TM + HALO
NT = T // 128
NE = 16
DE = 512
ALPHA = float((2 * L) ** 0.25)
LN_EPS = 1e-5
RMS_EPS = 1e-6
NB = 512
XNB = 512
XBLOCKS = [(0, 128)] + [(128 + i * XNB, XNB) for i in range(TM // XNB)]
MBLOCKS = [(0, 128)] + [(128 + i * NB, NB) for i in range(TM // NB)]
XOFF = []
_o = 0
for (_t0, _nb) in XBLOCKS:
    XOFF.append(_o)
    _o += 8 * _nb
XT_COLS = _o
NPP = 86
PP_DW = 0
PP_DWB = 62
PP_CLG = 64
PP_CLB = 66
PP_PSC = 68
PP_SCW = 70
PP_MG = 76
PP_BS = 82


class Ref:
    __slots__ = ("ap", "buf", "box")

    def __init__(self, ap, buf, box):
        self.ap = ap
        self.buf = buf
        self.box = box

    def v(self, fn):
        return Ref(fn(self.ap), self.buf, self.box)


class Buf:
    def __init__(self, handle, name, P, F, dtype):
        self.h = handle
        self.name = name
        self.P = P
        self.F = F
        self.dtype = dtype
        self.esz = _DSZ[dtype]
        self.entries = {}
        self.whole = False

    def __call__(self, f0=0, f1=None, p0=0, p1=None):
        if f1 is None:
            f1 = self.F
        if p1 is None:
            p1 = self.P
        assert 0 <= f0 < f1 <= self.F and 0 <= p0 < p1 <= self.P, (self.name, f0, f1, p0, p1)
        if self.whole:
            return Ref(self.h[p0:p1, f0:f1], self, (0, self.P, 0, self.F * self.esz))
        return Ref(self.h[p0:p1, f0:f1], self, (p0, p1, f0 * self.esz, f1 * self.esz))


def _ov(a, b):
    return a[0] < b[1] and b[0] < a[1] and a[2] < b[3] and b[2] < a[3]


def _cov(o, i):
    return o[0] <= i[0] and i[1] <= o[1] and o[2] <= i[2] and i[3] <= o[3]


class Eng:
    def __init__(self, name, eng):
        self.name = name
        self.e = eng
        self.sem = name
        self.ticket = 0
        self.seen = {}


class Emitter:
    def __init__(self, nc, sems):
        self.nc = nc
        self.sems = sems
        self.pe = Eng("pe", nc.tensor)
        self.act = Eng("act", nc.scalar)
        self.dve = Eng("dve", nc.vector)
        self.pool = Eng("pool", nc.gpsimd)
        self.sp = Eng("sp", nc.sync)
        self.dma_count = {}
        self.n_inst = 0
        self.n_wait = 0
        self.rec = None

    def _collect(self, E, reads, writes, is_dma):
        need = {}
        for r in reads:
            rb = r.box
            for (box, sk, w), val in r.buf.entries.items():
                if w and _ov(box, rb) and need.get(sk, 0) < val:
                    need[sk] = val
        for wr in writes:
            wb = wr.box
            for (box, sk, w), val in wr.buf.entries.items():
                if _ov(box, wb):
                    if sk == E.sem and E.name == "pe" and not is_dma:
                        continue
                    if need.get(sk, 0) < val:
                        need[sk] = val
        return need

    def _waits(self, E, need):
        for sk, val in need.items():
            if E.seen.get(sk, 0) >= val:
                continue
            E.e.wait_ge(self.sems[sk], val)
            E.seen[sk] = val
            self.n_wait += 1

    def _record(self, reads, writes, sk, val):
        for wr in writes:
            ent = wr.buf.entries
            wb = wr.box
            dead = [k for k in ent if _cov(wb, k[0])]
            for k in dead:
                del ent[k]
            ent[(wb, sk, True)] = val
        for r in reads:
            r.buf.entries[(r.box, sk, False)] = val

    def replay(self, rec):
        self.op(rec[0], rec[1], rec[2], rec[3])

    def op(self, E, fn, w=(), r=()):
        if self.rec is not None:
            self.rec.append((E, fn, tuple(w), tuple(r)))
            return None
        self._waits(E, self._collect(E, r, w, False))
        ins = fn(E.e)
        E.ticket += 1
        ins.then_inc(self.sems[E.sem], 1)
        self._record(r, w, E.sem, E.ticket)
        self.n_inst += 1
        return ins

    def dma(self, E, out, in_, semkey, w=(), r=(), group_left=0):
        self._waits(E, self._collect(E, r, w, True))
        ins = E.e.dma_start(out=out, in_=in_)
        c = self.dma_count.get(semkey, 0) + 16
        self.dma_count[semkey] = c
        ins.then_inc(self.sems[semkey], 16)
        self._record(r, w, semkey, c + 16 * group_left)
        self.n_inst += 1
        return ins


def build(n_layers=L, stop=None):
    nc = bass.Bass("TRN2", target_bir_lowering=False)

    def din(name, shape):
        return nc.dram_tensor(name, list(shape), F32, kind="ExternalInput").ap()

    x_d = din("x", [T, D])
    ident_d = din("ident", [128, 128])
    maskT_d = din("maskT", [128, 128])
    rcfix_d = din("rcfix", [128, 32])
    invw_d = din("invw", [128, 2])
    hflag_d = din("hflag", [128, 1])
    rbb_d = din("rbb", [128, NE])
    pp_d = din("pp", [L, 128, NPP])
    bc3_d = din("bc3", [L, 128, 768])
    lnp_d = din("lnp", [L, 128, 4096])
    gmwT_d = din("gmwT", [L, 128, 512])
    pbd_d = din("pbd", [L, 256, 128])
    w_in_d = din("w_in", [L, D, 2048])
    w_o_d = din("w_o", [L, D, D])
    cf_pw_d = din("cf_pw", [L, 256, 256])
    rw_d = din("router_w", [D, NE])
    wg_d = din("exp_w_gate", [L, NE, D, DE])
    wu_d = din("exp_w_up", [L, NE, D, DE])
    wd_d = din("exp_w_down", [L, NE, DE, D])
    out_d = nc.dram_tensor("out", [TM, D], F32, kind="ExternalOutput").ap()

    with ExitStack() as st:
        def sb(name, F, dt=F32, P=128):
            h = st.enter_context(nc.sbuf_tensor("s_" + name, [P, F], dt))
            return Buf(h, name, P, F, dt)

        def ps(name, F, dt=F32):
            h = st.enter_context(nc.psum_tensor(name, [128, F], dt))
            bf = Buf(h, name, 128, F, dt)
            bf.whole = True
            return bf

        semnames = ["pe", "act", "dve", "pool", "sp", "dx", "dc", "dpar", "dwin", "dwo",
                    "dwg0", "dwu0", "dwd0", "dwg1", "dwu1", "dwd1", "dout", "dln2", "dcp", "dparp"]
        sems = {k: st.enter_context(nc.semaphore(k)) for k in semnames}
        em = Emitter(nc, sems)
        PE, ACT, DVE, POOL, SP = em.pe, em.act, em.dve, em.pool, em.sp

        xtok = sb("xtok", NT * D)
        xT = sb("xT", XT_COLS, BF16)
        wbuf = sb("wbuf", 24576, BF16)
        lnp = sb("lnp", 2048)
        bc3 = sb("bc3", 768)
        ppb = sb("ppb", NPP)
        identf = sb("identf", 128)
        identb = sb("identb", 128, BF16)
        maskT = sb("maskT", 128)
        ones = sb("ones", 128)
        rcfix = sb("rcfix", 32)
        invw = sb("invw", 2)
        hflag = sb("hflag", 1)
        rbb = sb("rbb", NE)
        wmT = sb("wmT", 512, BF16)
        pwb = sb("pwb", 512, BF16)
        pbd = sb("pbd", 256, BF16)
        rwb = sb("rwb", 8 * NE, BF16)
        gtmp = sb("gtmp", 512)

        def gates(a=0, c=NT * NE):
            return gtmp(a, c)
        yT = sb("yT", 8 * NB, BF16)
        SW = NB + 32
        NS = 9
        ZL0, CH0, TM0 = 0, 2, 4
        scr = sb("scr", NS * SW)
        scrb = sb("scrb", 2 * 4 * XNB, BF16)
        HBW = 30 + NB + 2
        hbb = sb("hbb", 2 * HBW, BF16)
        dg = sb("dg", 4 * 128, BF16)
        sm = sb("sm", 384)

        def rt(a, c):
            return scr(2 * SW + a, 2 * SW + c)

        def SG(k, nb):
            return scr(k * SW, k * SW + nb)

        xbs = [(lambda a=0, c=D, k=k: scrb(2048 + k * D + a, 2048 + k * D + c)) for k in range(2)]

        def wmS(a=0, c=512):
            return scr(7 * SW + a, 7 * SW + c)

        ACC = [lambda a, c, p0=0, p1=128, k=k: scr((7 + k) * SW + a, (7 + k) * SW + c, p0, p1) for k in range(2)]

        def S(i, f0=0, f1=SW, p0=0, p1=128):
            return scr(i * SW + f0, i * SW + f1, p0, p1)

        B = [ps("pb%d" % i, 512) for i in range(6)]
        pTs = [ps("pT%d" % i, 1024, BF16) for i in range(2)]

        def mm(out, lhsT, rhs, start, stop):
            em.op(PE, lambda e: e.matmul(out.ap, lhsT.ap, rhs.ap, start=start, stop=stop), w=[out], r=[lhsT, rhs])

        def act(out, in_, func, bias=None, scale=None, extra_r=()):
            kw = {}
            if bias is not None:
                kw["bias"] = bias.ap if isinstance(bias, Ref) else bias
            if scale is not None:
                kw["scale"] = scale.ap if isinstance(scale, Ref) else scale
            rr = [in_] + [a for a in (bias, scale) if isinstance(a, Ref)] + list(extra_r)
            em.op(ACT, lambda e: e.activation(out=out.ap, in_=in_.ap, func=func, **kw), w=[out], r=rr)

        def tt(E, out, a, b, op):
            em.op(E, lambda e: e.tensor_tensor(out=out.ap, in0=a.ap, in1=b.ap, op=op), w=[out], r=[a, b])

        def ts(E, out, a, s1, op0, s2=None, op1=None):
            rr = [a] + [s for s in (s1, s2) if isinstance(s, Ref)]
            a1 = s1.ap if isinstance(s1, Ref) else s1
            a2 = s2.ap if isinstance(s2, Ref) else s2
            if op1 is None:
                em.op(E, lambda e: e.tensor_scalar(out=out.ap, in0=a.ap, scalar1=a1, scalar2=None, op0=op0), w=[out], r=rr)
            else:
                em.op(E, lambda e: e.tensor_scalar(out=out.ap, in0=a.ap, scalar1=a1, scalar2=a2, op0=op0, op1=op1),
                      w=[out], r=rr)

        def stt(E, out, a, s, b, op0, op1):
            rr = [a, b] + ([s] if isinstance(s, Ref) else [])
            sv = s.ap if isinstance(s, Ref) else s
            em.op(E, lambda e: e.scalar_tensor_tensor(out=out.ap, in0=a.ap, scalar=sv, in1=b.ap, op0=op0, op1=op1),
                  w=[out], r=rr)

        def cpy(E, out, in_):
            if E is ACT:
                em.op(E, lambda e: e.copy(out=out.ap, in_=in_.ap), w=[out], r=[in_])
            else:
                em.op(E, lambda e: e.tensor_copy(out=out.ap, in_=in_.ap), w=[out], r=[in_])

        def powm(out, in_, n):
            act(out, in_, AF.Ln)
            act(out, out, AF.Exp, scale=-0.5)

        def xloc(t0):
            if t0 < 128:
                return 0, t0
            return 1 + (t0 - 128) // XNB, (t0 - 128) % XNB

        def xT_tok(t0, n, kc):
            xb_, c0 = xloc(t0)
            nbx = XBLOCKS[xb_][1]
            assert c0 + n <= nbx
            return xT(XOFF[xb_] + kc * nbx + c0, XOFF[xb_] + kc * nbx + c0 + n)

        def xtile(i, f0=0, f1=D):
            return xtok(i * D + f0, i * D + f1)

        def pipeline(items, stages):
            n, m = len(items), len(stages)
            for step in range(n + m - 1):
                for k in range(m - 1, -1, -1):
                    idx = step - k
                    if 0 <= idx < n:
                        stages[k](idx, items[idx])

        def SMT(i, a, c):
            return sm(i * 20 + a, i * 20 + c)

        def ln_stages(goff, ring0=0):
            def sA(r, i):
                for c in range(2):
                    em.op(DVE, lambda e, c=c: e.bn_stats(out=SMT(i, c * 6, c * 6 + 6).ap, in_=xtile(i, c * 512, (c + 1) * 512).ap),
                          w=[SMT(i, c * 6, c * 6 + 6)], r=[xtile(i, c * 512, (c + 1) * 512)])
                em.op(DVE, lambda e: e.bn_aggr(out=SMT(i, 12, 14).ap, in_=SMT(i, 0, 12).ap), w=[SMT(i, 12, 14)],
                      r=[SMT(i, 0, 12)])
                ts(DVE, SMT(i, 14, 15), SMT(i, 13, 14), LN_EPS, ALU.add)

            def sB(r, i):
                powm(SMT(i, 15, 16), SMT(i, 14, 15), 1)
                stt(DVE, SMT(i, 16, 17), SMT(i, 12, 13), -1.0, SMT(i, 15, 16), ALU.mult, ALU.mult)

            def sC(r, i):
                act(xtile(i), xtile(i), AF.Identity, bias=SMT(i, 16, 17), scale=SMT(i, 15, 16))

            def sD(r, i):
                tt(POOL, xtile(i), xtile(i), lnp(goff, goff + D), ALU.mult)
                tt(POOL, xtile(i), xtile(i), lnp(goff + D, goff + 2 * D), ALU.add)
            return [sA, sB, sC, sD]

        def xT_stages(scale0=None, alpha_after=False, pt_fixed=None):
            def sE(r, i):
                xb = xbs[r % 2]
                if scale0 is not None and i == 0:
                    act(xb(), xtile(i), AF.Identity, scale=scale0)
                else:
                    cpy(ACT, xb(), xtile(i))
                if alpha_after:
                    em.op(ACT, lambda e: e.mul(out=xtile(i).ap, in_=xtile(i).ap, mul=ALPHA), w=[xtile(i)], r=[xtile(i)])

            def sF(r, i):
                xb = xbs[r % 2]
                pT = pTs[r % 2 if pt_fixed is None else pt_fixed]
                for kc in range(8):
                    em.op(PE, lambda e, kc=kc: e.transpose(out=pT(kc * 128, (kc + 1) * 128).ap, in_=xb(kc * 128, (kc + 1) * 128).ap,
                                                    identity=identb().ap),
                          w=[pT(kc * 128, (kc + 1) * 128)], r=[xb(kc * 128, (kc + 1) * 128), identb()])

            def sG(r, i):
                pT = pTs[r % 2 if pt_fixed is None else pt_fixed]
                b, c0 = xloc(i * 128)
                nb = XBLOCKS[b][1]
                dst = xT(XOFF[b], XOFF[b] + 8 * nb).v(
                    lambda ap: ap.rearrange("p (k t) -> p k t", k=8)[:, :, c0:c0 + 128])
                src = pT().v(lambda ap: ap.rearrange("p (k t) -> p k t", k=8))
                cpy(ACT, dst, src)
            return [sE, sF, sG]

        def rms_group(yf, nb, gcol, c0):
            sq = S(TM0 + 2, 0, nb)
            for j in range(2):
                act(sq, yf[j], AF.Square)
                mm(B[4](0, nb), ones(), sq, j == 0, j == 1)
            ve = S(TM0 + 1, 0, nb)
            ts(DVE, ve, B[4](0, nb), 1.0 / 256.0, ALU.mult, RMS_EPS, ALU.add)
            powm(ve, ve, nb)
            for j in range(2):
                stt(DVE, yT((c0 + j) * nb, (c0 + j + 1) * nb), yf[j], ppb(gcol + j, gcol + j + 1), ve, ALU.mult, ALU.mult)

        for i in range(NT):
            em.dma(SP, xtile(i).ap, x_d[i * 128:(i + 1) * 128, :], "dx", w=[xtile(i)], group_left=NT - 1 - i)
        cl = [(identf, ident_d), (maskT, maskT_d), (rcfix, rcfix_d), (invw, invw_d), (hflag, hflag_d), (rbb, rbb_d)]
        for n, (bf, d) in enumerate(cl):
            em.dma(SP, bf().ap, d, "dc", w=[bf()], group_left=len(cl) - 1 - n)
        em.dma(POOL, rwb().ap.rearrange("p (k n) -> p k n", k=8), rw_d.rearrange("(k p) n -> p k n", p=128), "dcp",
               w=[rwb()])
        cpy(ACT, identb(), identf())
        em.op(DVE, lambda e: e.memset(ones().ap, 1.0), w=[ones()])

        def load_layer_weights(l):
            for q in range(4):
                em.dma(POOL, wbuf(q * 4096, (q + 1) * 4096).ap.rearrange("p (k n) -> p k n", k=2),
                       w_in_d[l, q * 256:(q + 1) * 256, :].rearrange("(k p) n -> p k n", p=128), "dwin",
                       w=[wbuf(q * 4096, (q + 1) * 4096)], group_left=3 - q)
            for q in range(2):
                em.dma(POOL, wbuf(16384 + q * 4096, 16384 + (q + 1) * 4096).ap.rearrange("p (k n) -> p k n", k=4),
                       w_o_d[l, q * 512:(q + 1) * 512, :].rearrange("(k p) n -> p k n", p=128), "dwo",
                       w=[wbuf(16384 + q * 4096, 16384 + (q + 1) * 4096)], group_left=1 - q)

        def load_layer_params(l):
            lst = [(SP, ppb(), pp_d[l]), (SP, bc3(), bc3_d[l]), (SP, lnp(), lnp_d[l, :, 0:2048]), (SP, wmS(), gmwT_d[l])]
            n = len(lst)
            for k, (E, rf, d) in enumerate(lst):
                em.dma(E, rf.ap, d, "dpar", w=[rf], group_left=n - 1 - k)
            em.dma(POOL, pwb().ap.rearrange("p (k n) -> p k n", k=2),
                   cf_pw_d[l].rearrange("(k p) n -> p k n", p=128), "dparp", w=[pwb()], group_left=1)
            em.dma(POOL, pbd().ap.rearrange("p (k n) -> p k n", k=2),
                   pbd_d[l].rearrange("(k p) n -> p k n", p=128), "dparp", w=[pbd()], group_left=0)
            tt(DVE, wmT().v(lambda ap: ap.rearrange("p (h t) -> p h t", h=4)),
               wmS().v(lambda ap: ap.rearrange("p (h t) -> p h t", h=4)),
               maskT().v(lambda ap: ap.rearrange("p (o t) -> p o t", o=1).to_broadcast([128, 4, 128])), ALU.mult)

        def load_expert(l, e):
            s = e % 2
            base = s * 12288
            em.dma(POOL, wbuf(base, base + 4096).ap.rearrange("p (k n) -> p k n", k=8),
                   wg_d[l, e].rearrange("(k p) n -> p k n", p=128), "dwg%d" % s, w=[wbuf(base, base + 4096)])
            em.dma(POOL, wbuf(base + 4096, base + 8192).ap.rearrange("p (k n) -> p k n", k=8),
                   wu_d[l, e].rearrange("(k p) n -> p k n", p=128), "dwu%d" % s, w=[wbuf(base + 4096, base + 8192)])
            em.dma(POOL, wbuf(base + 8192, base + 12288).ap.rearrange("p (k n) -> p k n", k=4),
                   wd_d[l, e].rearrange("(k p) n -> p k n", p=128), "dwd%d" % s, w=[wbuf(base + 8192, base + 12288)])

        load_layer_weights(0)
        load_layer_params(0)
        pipeline(list(range(NT)), xT_stages())

        def win(kc, c0, c1):
            return wbuf(kc * 2048 + c0, kc * 2048 + c1)

        def wo(kc, c0, c1):
            return wbuf(16384 + kc * 1024 + c0, 16384 + kc * 1024 + c1)

        zrot = [0]
        dgi = [0]
        pre_e0 = [False]

        def zbank():
            b = B[2 + zrot[0] % 2]
            zrot[0] += 1
            return b

        def zmm(bank, t0, nb, col0):
            for kc in range(8):
                mm(bank(0, nb), win(kc, col0, col0 + 128), xT_tok(t0, nb, kc), kc == 0, kc == 7)

        def finish_dump():
            for i in range(1, NT):
                em.dma(SP, out_d[(i - 1) * 128:i * 128, :], xtile(i).ap, "dout", r=[xtile(i)])
            SP.e.wait_ge(sems["dout"], em.dma_count["dout"])
            build.stats = (em.n_inst, em.n_wait)

        if stop == "setup":
            finish_dump()
            return nc
        for l in range(n_layers):
            for j in range(2):
                em.op(DVE, lambda e: e.memset(hbb(j * HBW, j * HBW + 30).ap, 0.0), w=[hbb(j * HBW, j * HBW + 30)])
                em.op(DVE, lambda e: e.memset(S(ZL0 + j, 0, 15).ap, 0.0), w=[S(ZL0 + j, 0, 15)])
                em.op(DVE, lambda e: e.memset(S(CH0 + j, 0, 2).ap, 0.0), w=[S(CH0 + j, 0, 2)])

            last = (l == n_layers - 1)
            def G_section(b):
                t0, nb = MBLOCKS[b]
                tiles = list(range(t0 // 128, (t0 + nb) // 128))
                state_only = last and b == 0 and stop is None
                GS = 340

                def g_bufs(r):
                    p = r % 2
                    p = 0
                    pUV, pS_ = B[0], B[1]
                    sl = [(lambda a, c: gtmp(a, c)), (lambda a, c: gtmp(256 + a, 256 + c)), (lambda a, c: gtmp(a, c))]
                    u_sb, vn, yg = sl[0](0, 256), sl[1](0, 256), sl[2](0, 256)
                    q = lambda a, c: sm(GS + a, GS + c)
                    vb = lambda a, c: scrb(1024 + a, 1024 + c)
                    return pUV, pS_, sl, u_sb, vn, yg, q, vb, pTs[1]

                def g0(r, i):
                    pUV = g_bufs(r)[0]
                    for kc in range(8):
                        mm(pUV(), xT_tok(i * 128, 128, kc), win(kc, 0, 512), kc == 0, kc == 7)

                def g1(r, i):
                    pUV, pS_, sl, u_sb, vn, yg, q, vb, pT = g_bufs(r)
                    cpy(ACT, u_sb, pUV(0, 256))
                    cpy(ACT, vn, pUV(256, 512))

                def g2(r, i):
                    pUV, pS_, sl, u_sb, vn, yg, q, vb, pT = g_bufs(r)
                    em.op(DVE, lambda e: e.bn_stats(out=q(0, 6).ap, in_=vn.ap), w=[q(0, 6)], r=[vn])
                    em.op(DVE, lambda e: e.bn_aggr(out=q(6, 8).ap, in_=q(0, 6).ap), w=[q(6, 8)], r=[q(0, 6)])
                    ts(DVE, q(8, 9), q(7, 8), LN_EPS, ALU.add)

                def g3(r, i):
                    pUV, pS_, sl, u_sb, vn, yg, q, vb, pT = g_bufs(r)
                    powm(q(9, 10), q(8, 9), 1)

                def g4(r, i):
                    pUV, pS_, sl, u_sb, vn, yg, q, vb, pT = g_bufs(r)
                    ts(DVE, vn, vn, q(6, 7), ALU.subtract, q(9, 10), ALU.mult)
                    tt(DVE, vn, vn, bc3(0, 256), ALU.mult)
                    tt(DVE, vb(0, 256), vn, bc3(256, 512), ALU.add)

                def g5(r, i):
                    pUV, pS_, sl, u_sb, vn, yg, q, vb, pT = g_bufs(r)
                    for h in range(4):
                        mm(pS_(64 * h, 64 * h + 64), wmT(128 * h, 128 * h + 128), vb(64 * h, 64 * h + 64), True, True)

                def g6(r, i):
                    pUV, pS_, sl, u_sb, vn, yg, q, vb, pT = g_bufs(r)
                    for h in range(4):
                        stt(DVE, sl[2](64 * h, 64 * h + 64), pS_(64 * h, 64 * h + 64), ppb(PP_BS + h, PP_BS + h + 1),
                            sl[0](64 * h, 64 * h + 64), ALU.add, ALU.mult)
                    em.op(DVE, lambda e: e.bn_stats(out=q(10, 16).ap, in_=yg.ap), w=[q(10, 16)], r=[yg])
                    em.op(DVE, lambda e: e.bn_aggr(out=q(16, 18).ap, in_=q(10, 16).ap), w=[q(16, 18)], r=[q(10, 16)])
                    stt(DVE, q(18, 19), q(16, 17), q(16, 17), q(17, 18), ALU.mult, ALU.add)
                    ts(DVE, q(18, 19), q(18, 19), RMS_EPS, ALU.add)

                def g7(r, i):
                    pUV, pS_, sl, u_sb, vn, yg, q, vb, pT = g_bufs(r)
                    powm(q(19, 20), q(18, 19), 1)
                    stt(DVE, vb(256, 512), yg, q(19, 20), bc3(512, 768), ALU.mult, ALU.mult)

                def g8(r, i):
                    pUV, pS_, sl, u_sb, vn, yg, q, vb, pT = g_bufs(r)
                    for c in range(2):
                        em.op(PE, lambda e, c=c: e.transpose(out=pT(c * 128, (c + 1) * 128).ap,
                                                        in_=vb(256 + c * 128, 256 + (c + 1) * 128).ap,
                                                        identity=identb().ap),
                              w=[pT(c * 128, (c + 1) * 128)], r=[vb(256 + c * 128, 256 + (c + 1) * 128), identb()])

                def g9(r, i):
                    pT = g_bufs(r)[8]
                    c0 = i * 128 - t0
                    dst = yT(0, 2 * nb).v(lambda ap: ap.rearrange("p (k t) -> p k t", k=2)[:, :, c0:c0 + 128])
                    cpy(ACT, dst, pT(0, 256).v(lambda ap: ap.rearrange("p (k t) -> p k t", k=2)))

                for i_ in tiles:
                    for gs in [g0, g1, g2, g3, g4, g5, g6, g7, g8, g9]:
                        gs(0, i_)


            def CPS_section(b):
                t0, nb = MBLOCKS[b]
                tiles = list(range(t0 // 128, (t0 + nb) // 128))
                state_only = last and b == 0 and stop is None
                accs = []
                for j in range(2):
                    pa = zbank()
                    zmm(pa, t0, nb, 512 + j * 128)
                    pg = zbank()
                    zmm(pg, t0, nb, 768 + j * 128)
                    sig = S(TM0 + j, 0, nb)
                    act(sig, pg(0, nb), AF.Tanh, scale=0.5)
                    hb = lambda a, c, j=j: hbb(j * HBW + a, j * HBW + c)
                    stt(DVE, hb(30, 30 + nb), sig, 1.0, pa(0, nb), ALU.add, ALU.mult)
                hbs = [(lambda a, c, j=j: hbb(j * HBW + a, j * HBW + c)) for j in range(2)]
                if state_only:
                    for j in range(2):
                        cpy(ACT, hbs[j](0, 30), hbs[j](nb, nb + 30))
                else:
                    pcs = [zbank(), zbank()]
                    for k in range(31):
                        for j in range(2):
                            slot = dgi[0] % 4
                            dgi[0] += 1
                            wcol = ppb(PP_DW + j * 31 + k, PP_DW + j * 31 + k + 1)
                            GE = (POOL, ACT, POOL, ACT)[slot]
                            if GE is ACT:
                                act(dg(slot * 128, (slot + 1) * 128), identf(), AF.Identity, scale=wcol)
                            elif GE is DVE:
                                ts(GE, dg(slot * 128, (slot + 1) * 128), identf(), wcol, ALU.mult)
                            else:
                                ts(GE, dg(slot * 128, (slot + 1) * 128), identf(), wcol, ALU.mult, 0.0, ALU.add)
                            mm(pcs[j](0, nb), dg(slot * 128, (slot + 1) * 128), hbs[j](k, k + nb), k == 0, k == 30)
                    for j in range(2):
                        act(ACC[j](0, nb), pcs[j](0, nb), AF.Identity, bias=ppb(PP_DWB + j, PP_DWB + j + 1), scale=0.5)
                    for j in range(2):
                        cpy(ACT, hbs[j](0, 30), hbs[j](nb, nb + 30))
                        accs.append(ACC[j](0, nb))
                if state_only:
                    for j in range(2):
                        pz = zbank()
                        zmm(pz, t0, nb, 1024 + j * 128)
                        cpy(ACT, S(ZL0 + j, 15, 15 + nb), pz(0, nb))
                        cpy(ACT, S(ZL0 + j, 0, 15), S(ZL0 + j, nb, nb + 15))
                        pC_ = zbank()
                        zmm(pC_, t0, nb, 1536 + j * 128)
                        pH_ = zbank()
                        zmm(pH_, t0, nb, 1792 + j * 128)
                        cs = S(TM0, 0, nb)
                        cpy(ACT, cs, pC_(0, nb))
                        tt(DVE, S(CH0 + j, 2, 2 + nb), pH_(0, nb), cs, ALU.mult)
                        cpy(ACT, S(CH0 + j, 0, 2), S(CH0 + j, nb, nb + 2))
                    return
                sq = S(TM0 + 2, 0, nb)
                for j in range(2):
                    act(sq, accs[j], AF.Square)
                    mm(B[5](0, nb), ones(), accs[j], j == 0, j == 1)
                    mm(B[4](0, nb), ones(), sq, j == 0, j == 1)
                mean = S(TM0, 0, nb)
                ts(DVE, mean, B[5](0, nb), 1.0 / 256.0, ALU.mult)
                msq = S(TM0 + 1, 0, nb)
                tt(DVE, msq, mean, mean, ALU.mult)
                ve = S(TM0 + 2, 0, nb)
                ts(DVE, ve, B[4](0, nb), 1.0 / 256.0, ALU.mult, LN_EPS, ALU.add)
                tt(DVE, ve, ve, msq, ALU.subtract)
                powm(ve, ve, nb)
                for j in range(2):
                    tt(DVE, accs[j], accs[j], mean, ALU.subtract)
                    tt(DVE, accs[j], accs[j], ve, ALU.mult)
                    act(scrb(j * nb, (j + 1) * nb), accs[j], AF.Silu, bias=ppb(PP_CLB + j, PP_CLB + j + 1),
                        scale=ppb(PP_CLG + j, PP_CLG + j + 1))
                yf = []
                for jo in range(2):
                    for j in range(2):
                        mm(B[5](0, nb), pwb(j * 256 + jo * 128, j * 256 + (jo + 1) * 128), scrb(j * nb, (j + 1) * nb),
                           j == 0, j == 1)
                    cpy(ACT, ACC[jo](0, nb), B[5](0, nb))
                    yf.append(ACC[jo](0, nb))
                rms_group(yf, nb, PP_MG + 0, 2)

                zls = [(lambda a, c, p0=0, p1=128, j=j: S(ZL0 + j, a, c, p0, p1)) for j in range(2)]
                Xs = [(lambda a, c, p0=0, p1=128: S(TM0, a, c, p0, p1)), (lambda a, c, p0=0, p1=128: S(TM0 + 2, a, c, p0, p1))]
                Ys = [(lambda a, c, p0=0, p1=128: S(TM0 + 1, a, c, p0, p1)), ACC[1]]
                pls = [(lambda a, c, p0=0, p1=128, j=j: scrb(j * nb + a, j * nb + c, p0, p1)) for j in range(2)]
                pzb = [B[2], B[3]]
                pob = [B[5], B[4]]
                for j in range(2):
                    zmm(pzb[j], t0, nb, 1024 + j * 128)
                for j in range(2):
                    cpy(ACT, zls[j](15, 15 + nb), pzb[j](0, nb))
                for j in range(2):
                    tt(DVE, Xs[j](1, 15 + nb), zls[j](1, 15 + nb), zls[j](0, 14 + nb), ALU.add)
                for j in range(2):
                    tt(DVE, Ys[j](3, 15 + nb), Xs[j](3, 15 + nb), Xs[j](1, 13 + nb), ALU.add)
                tt(DVE, Xs[1](7, 15 + nb), Ys[1](7, 15 + nb), Ys[1](3, 11 + nb), ALU.add)
                tt(DVE, Ys[1](15, 15 + nb), Xs[1](15, 15 + nb), Xs[1](7, 7 + nb), ALU.add)
                for (p0, p1) in ((0, 64), (64, 128)):
                    for j in range(2):
                        src = Xs[j] if p0 == 0 else Ys[j]
                        stt(DVE, pls[j](0, nb, p0, p1), src(15, 15 + nb, p0, p1), invw(j, j + 1, p0, p1),
                            zls[j](15, 15 + nb, p0, p1), ALU.mult, ALU.subtract)
                if b == 1:
                    for j in range(2):
                        for (p0, p1) in ((0, 64), (64, 128)):
                            src = Xs[j] if p0 == 0 else Ys[j]
                            tmp = sm(368, 384, p0, p1)
                            tt(DVE, tmp, src(15, 31, p0, p1), rcfix(j * 16, j * 16 + 16, p0, p1), ALU.mult)
                            tt(DVE, pls[j](0, 16, p0, p1), tmp, zls[j](15, 31, p0, p1), ALU.subtract)
                for j in range(2):
                    cpy(ACT, zls[j](0, 15), zls[j](nb, nb + 15))
                for j in range(2):
                    mm(pob[j](0, nb), pbd(j * 128, (j + 1) * 128), pls[j](0, nb), True, True)
                for j in range(2):
                    act(ACC[j](0, nb), pob[j](0, nb), AF.Identity, scale=ppb(PP_PSC + j, PP_PSC + j + 1))
                rms_group([ACC[0](0, nb), ACC[1](0, nb)], nb, PP_MG + 2, 4)

                def zmm_to(bank, col0):
                    zmm(bank, t0, nb, col0)
                    return bank
                sbank = [(B[2], B[3], B[2]), (B[4], B[5], B[4])]
                chls = [(lambda a, c, j=j: S(CH0 + j, a, c)) for j in range(2)]
                css = [S(TM0 + j, 0, nb) for j in range(2)]
                for j in range(2):
                    zmm_to(sbank[j][0], 1536 + j * 128)
                    zmm_to(sbank[j][1], 1792 + j * 128)
                for j in range(2):
                    cpy(ACT, css[j], sbank[j][0](0, nb))
                for j in range(2):
                    tt(DVE, chls[j](2, 2 + nb), sbank[j][1](0, nb), css[j], ALU.mult)
                for j in range(2):
                    zmm_to(sbank[j][2], 1280 + j * 128)
                for j in range(2):
                    ts(DVE, ACC[j](0, nb), chls[j](0, nb), ppb(PP_SCW + j * 3, PP_SCW + j * 3 + 1), ALU.mult)
                for k in (1, 2):
                    for j in range(2):
                        stt(DVE, ACC[j](0, nb), chls[j](k, k + nb), ppb(PP_SCW + j * 3 + k, PP_SCW + j * 3 + k + 1),
                            ACC[j](0, nb), ALU.mult, ALU.add)
                for j in range(2):
                    tt(DVE, ACC[j](0, nb), sbank[j][2](0, nb), ACC[j](0, nb), ALU.mult)
                for j in range(2):
                    cpy(ACT, chls[j](0, 2), chls[j](nb, nb + 2))
                rms_group([ACC[0](0, nb), ACC[1](0, nb)], nb, PP_MG + 4, 6)


            def O0_section(b):
                t0, nb = MBLOCKS[b]
                tiles = list(range(t0 // 128, (t0 + nb) // 128))
                for r, i in enumerate(tiles):
                    c0 = i * 128 - t0
                    banks = (B[0], B[1]) if r % 2 == 0 else (B[2], B[3])
                    for nh in range(2):
                        for kc in range(8):
                            mm(banks[nh](), yT(kc * nb + c0, kc * nb + c0 + 128), wo(kc, nh * 512, (nh + 1) * 512),
                               kc == 0, kc == 7)
                        stt(DVE, xtile(i, nh * 512, (nh + 1) * 512), xtile(i, nh * 512, (nh + 1) * 512), ALPHA, banks[nh](),
                            ALU.mult, ALU.add)

            def A_section(b):
                t0, nb = MBLOCKS[b]
                tiles = list(range(t0 // 128, (t0 + nb) // 128))
                pipeline(tiles, ln_stages(0) + xT_stages(alpha_after=True, pt_fixed=0))

            def recorded(fn, *a):
                em.rec = []
                fn(*a)
                r_, em.rec = em.rec, None
                return r_

            def interleave(ra, rb):
                na, nb_ = len(ra), len(rb)
                out, ia, ib = [], 0, 0
                while ia < na or ib < nb_:
                    if ib >= nb_ or (ia < na and ia * nb_ <= ib * na):
                        out.append(ra[ia]); ia += 1
                    else:
                        out.append(rb[ib]); ib += 1
                return out

            nblk = len(MBLOCKS)
            skip0 = last and stop is None
            if skip0:
                CPS_section(0)
            fb = 1 if skip0 else 0
            G_section(fb)
            CPS_section(fb)
            O0_section(fb)
            for b in range(fb, nblk):
                ra = recorded(A_section, b)
                if b + 1 < nblk:
                    x = interleave(ra, recorded(G_section, b + 1))
                    for rec in interleave(x, recorded(CPS_section, b + 1)):
                        em.replay(rec)
                    O0_section(b + 1)
                    pass
                else:
                    for rec in ra:
                        em.replay(rec)
            if stop == "mixer":
                finish_dump()
                return nc
            em.dma(SP, lnp().ap, lnp_d[l, :, 2048:4096], "dln2", w=[lnp()])
            if not pre_e0[0]:
                load_expert(l, 0)
            pre_e0[0] = False
            load_expert(l, 1)
            NL = NT * NE
            for i in range(NT):
                for kc in range(8):
                    mm(B[4](i * NE, (i + 1) * NE), xT_tok(i * 128, 128, kc), rwb(kc * NE, (kc + 1) * NE), kc == 0, kc == 7)
            lg = rt(0, NL)
            sel = rt(NL, 2 * NL)
            msk = rt(2 * NL, 3 * NL)
            eq1 = rt(3 * NL, 4 * NL)
            eq2 = rt(4 * NL, 5 * NL)
            tmp = rt(5 * NL, 6 * NL)
            pr = rt(3 * NL, 3 * NL + 68 * 6)
            o = 6 * NL
            gs = rt(o, o + 68)
            gmax = rt(o + 68, o + 85)
            m1 = rt(o + 85, o + 102)
            la = rt(o + 102, o + 119)
            lb = rt(o + 119, o + 136)
            g1 = rt(o + 136, o + 153)
            g2 = rt(o + 153, o + 170)
            v4 = lambda rf: rf.v(lambda ap: ap.rearrange("p (g f) -> p g f", f=4))
            v16 = lambda rf: rf.v(lambda ap: ap.rearrange("p (t e) -> p t e", e=NE))
            bc16 = lambda rf: rf.v(lambda ap: ap.rearrange("p (t o) -> p t o", o=1).to_broadcast([128, NT, NE]))
            cpy(DVE, lg, B[4](0, NL))
            tt(DVE, v16(sel), v16(lg),
               rbb().v(lambda ap: ap.rearrange("p (o e) -> p o e", o=1).to_broadcast([128, NT, NE])), ALU.add)
            s4v = sel.v(lambda ap: ap.rearrange("p (g f) -> p g f", f=4))
            p6 = lambda a, c: pr.v(lambda ap: ap.rearrange("p (g f) -> p g f", f=6)[:, :, a:c])
            s4s = lambda a, c: sel.v(lambda ap: ap.rearrange("p (g f) -> p g f", f=4)[:, :, a:c])
            tt(DVE, p6(0, 3), s4s(0, 3), s4s(1, 4), ALU.add)
            tt(DVE, p6(3, 5), s4s(0, 2), s4s(2, 4), ALU.add)
            tt(DVE, p6(5, 6), s4s(0, 1), s4s(3, 4), ALU.add)
            em.op(DVE, lambda e: e.reduce_max(out=gs.ap, in_=pr.ap.rearrange("p (g f) -> p g f", f=6), axis=AX.X),
                  w=[gs], r=[pr])
            em.op(DVE, lambda e: e.reduce_max(out=gmax.ap, in_=gs.ap.rearrange("p (t g) -> p t g", g=4), axis=AX.X),
                  w=[gmax], r=[gs])
            ing = rt(o + 170, o + 238)
            tt(DVE, ing.v(lambda ap: ap.rearrange("p (t g) -> p t g", g=4)),
               gs.v(lambda ap: ap.rearrange("p (t g) -> p t g", g=4)),
               gmax.v(lambda ap: ap.rearrange("p (t o) -> p t o", o=1).to_broadcast([128, NT, 4])), ALU.is_equal)
            ts(DVE, ing, ing, 1.0, ALU.subtract, 1e30, ALU.mult)
            tt(DVE, v4(msk), v4(sel),
               ing.v(lambda ap: ap.rearrange("p (g o) -> p g o", o=1).to_broadcast([128, 68, 4])), ALU.add)
            em.op(DVE, lambda e: e.reduce_max(out=m1.ap, in_=msk.ap.rearrange("p (t e) -> p t e", e=NE), axis=AX.X),
                  w=[m1], r=[msk])
            tt(DVE, v16(eq1), v16(msk), bc16(m1), ALU.is_equal)
            stt(DVE, msk, eq1, -1e30, msk, ALU.mult, ALU.add)
            em.op(DVE, lambda e: e.reduce_max(out=m1.ap, in_=msk.ap.rearrange("p (t e) -> p t e", e=NE), axis=AX.X),
                  w=[m1], r=[msk])
            tt(DVE, v16(eq2), v16(msk), bc16(m1), ALU.is_equal)
            tt(DVE, tmp, eq1, lg, ALU.mult)
            em.op(DVE, lambda e: e.reduce_sum(out=la.ap, in_=tmp.ap.rearrange("p (t e) -> p t e", e=NE), axis=AX.X),
                  w=[la], r=[tmp])
            tt(DVE, tmp, eq2, lg, ALU.mult)
            em.op(DVE, lambda e: e.reduce_sum(out=lb.ap, in_=tmp.ap.rearrange("p (t e) -> p t e", e=NE), axis=AX.X),
                  w=[lb], r=[tmp])
            tt(DVE, la, la, lb, ALU.subtract)
            act(g1, la, AF.Sigmoid)
            act(g2, la, AF.Sigmoid, scale=-1.0)
            tt(DVE, v16(eq1), v16(eq1), bc16(g1), ALU.mult)
            tt(DVE, v16(eq2), v16(eq2), bc16(g2), ALU.mult)
            tt(DVE, gates(), eq1, eq2, ALU.add)
            if stop == "router":
                finish_dump()
                return nc

            def ln2_block(tl):
                if l + 1 < n_layers:
                    pipeline(tl, ln_stages(0) + xT_stages(scale0=hflag()))
                else:
                    def sOut(r, i):
                        if i >= 1:
                            em.dma(SP, out_d[(i - 1) * 128:i * 128, :], xtile(i).ap, "dout", r=[xtile(i)])
                    pipeline(tl, ln_stages(0) + [sOut])

            pend_ln = []
            ln2_st = ln_stages(0)

            def ln2_out(r, i):
                if i >= 1:
                    em.dma(SP, out_d[(i - 1) * 128:i * 128, :], xtile(i).ap, "dout", r=[xtile(i)])

            it = 0
            for e_ in range(NE):
                spread_ln = (e_ == NE - 1) and stop is None
                s = e_ % 2
                base = s * 12288
                for b, (t0, nb) in enumerate(XBLOCKS):
                    if last and b == 0 and stop is None:
                        continue
                    tiles = list(range(t0 // 128, (t0 + nb) // 128))
                    hb0 = (it % 2) * 4 * XNB
                    for fc in range(4):
                        pG = B[(fc % 2) * 2]
                        pU = B[(fc % 2) * 2 + 1]
                        for kc in range(8):
                            mm(pG(0, nb), wbuf(base + kc * 512 + fc * 128, base + kc * 512 + (fc + 1) * 128),
                               xT_tok(t0, nb, kc), kc == 0, kc == 7)
                        for kc in range(8):
                            mm(pU(0, nb), wbuf(base + 4096 + kc * 512 + fc * 128, base + 4096 + kc * 512 + (fc + 1) * 128),
                               xT_tok(t0, nb, kc), kc == 0, kc == 7)
                        sg = SG(fc % 2, nb)
                        act(sg, pG(0, nb), AF.Silu)
                        tt(DVE, scrb(hb0 + fc * nb, hb0 + (fc + 1) * nb), sg, pU(0, nb), ALU.mult)
                        if spread_ln and pend_ln:
                            for r_, i_ in enumerate(pend_ln[0]):
                                ln2_st[fc](r_, i_)
                    if spread_ln and pend_ln:
                        for r_, i_ in enumerate(pend_ln.pop()):
                            if last:
                                ln2_out(r_, i_)
                    for i in tiles:
                        c0 = i * 128 - t0
                        for nh in range(2):
                            pY = B[4 + nh]
                            for fc in range(4):
                                mm(pY(), scrb(hb0 + fc * nb + c0, hb0 + fc * nb + c0 + 128),
                                   wbuf(base + 8192 + fc * 1024 + nh * 512, base + 8192 + fc * 1024 + (nh + 1) * 512),
                                   fc == 0, fc == 3)
                            stt(DVE, xtile(i, nh * 512, (nh + 1) * 512), pY(), gates(i * NE + e_, i * NE + e_ + 1),
                                xtile(i, nh * 512, (nh + 1) * 512), ALU.mult, ALU.add)
                    it += 1
                    if spread_ln:
                        pend_ln.append(tiles)
                if stop == "e%d" % e_:
                    finish_dump()
                    return nc
                if e_ + 2 < NE:
                    load_expert(l, e_ + 2)
            if l + 1 < n_layers:
                load_layer_weights(l + 1)
            while pend_ln:
                tl = pend_ln.pop()
                if last:
                    ln2_block(tl)
                else:
                    pipeline(tl, ln_stages(0))
            if stop is None and not last:
                pipeline(list(range(NT)), xT_stages(scale0=hflag()))
            if stop is not None:
                ln2_block([i for i in range(NT)])
            if l + 1 < n_layers:
                load_layer_params(l + 1)

        SP.e.wait_ge(sems["dout"], em.dma_count["dout"])
        build.stats = (em.n_inst, em.n_wait)
    return nc


_CACHE = {}


def _host_consts(half):
    ident = np.eye(128, dtype=np.float32)
    s = np.arange(128)
    maskT = (s[:, None] <= s[None, :]).astype(np.float32)
    wins = np.array([2, 4, 8, 16])
    p = np.arange(128)
    rcfix = np.zeros((128, 32), np.float32)
    invw = np.zeros((128, 2), np.float32)
    for j in range(2):
        w = wins[2 * j + (p // 64)].astype(np.float32)
        invw[:, j] = 1.0 / w
        t = np.arange(16, dtype=np.float32)[None, :]
        if half == 0:
            rcfix[:, j * 16:(j + 1) * 16] = 1.0 / np.minimum(t + 1.0, w[:, None])
        else:
            rcfix[:, j * 16:(j + 1) * 16] = 1.0 / w[:, None]
    hflag = np.full((128, 1), float(half), np.float32)
    return ident, maskT, rcfix, invw, hflag


def kernel(x, w_in, gm_ln_g, gm_ln_b, gm_w_s, gm_b_s, cf_dw_w, cf_dw_b, cf_ln_g, cf_ln_b, cf_pw, pool_w,
           pool_scale, sc_w, mix_norm_g, w_o, ln1_g, ln1_b, router_w, router_b, exp_w_gate, exp_w_up,
           exp_w_down, ln2_g, ln2_b):
    f = lambda a: np.ascontiguousarray(np.asarray(a, dtype=np.float32))
    x = f(x)
    pp = np.zeros((L, 128, NPP), np.float32)
    bc3 = np.zeros((L, 128, 768), np.float32)
    lnp = np.zeros((L, 128, 4096), np.float32)
    gmwT = np.zeros((L, 128, 512), np.float32)
    pbd = np.zeros((L, 256, 128), np.float32)
    cf_dw_w, cf_dw_b, cf_ln_g, cf_ln_b = f(cf_dw_w), f(cf_dw_b), f(cf_ln_g), f(cf_ln_b)
    pool_scale, sc_w, mix_norm_g, gm_b_s = f(pool_scale), f(sc_w), f(mix_norm_g), f(gm_b_s)
    gm_w_s, pool_w = f(gm_w_s), f(pool_w)
    for l in range(L):
        for j in range(2):
            ch = slice(j * 128, (j + 1) * 128)
            pp[l, :, PP_DW + j * 31:PP_DW + (j + 1) * 31] = cf_dw_w[l][:, ch].T
            pp[l, :, PP_DWB + j] = cf_dw_b[l][ch]
            pp[l, :, PP_CLG + j] = cf_ln_g[l][ch]
            pp[l, :, PP_CLB + j] = cf_ln_b[l][ch]
            pp[l, :, PP_PSC + j] = pool_scale[l][ch]
            pp[l, :, PP_SCW + j * 3:PP_SCW + (j + 1) * 3] = sc_w[l][:, ch].T
        for c in range(6):
            pp[l, :, PP_MG + c] = mix_norm_g[l][256 + c * 128:256 + (c + 1) * 128]
        pp[l, :, PP_BS:PP_BS + 4] = gm_b_s[l].T
        bc3[l, :, 0:256] = f(gm_ln_g)[l][None, :]
        bc3[l, :, 256:512] = f(gm_ln_b)[l][None, :]
        bc3[l, :, 512:768] = mix_norm_g[l][None, 0:256]
        lnp[l, :, 0:1024] = f(ln1_g)[l][None, :]
        lnp[l, :, 1024:2048] = f(ln1_b)[l][None, :]
        lnp[l, :, 2048:3072] = f(ln2_g)[l][None, :]
        lnp[l, :, 3072:4096] = f(ln2_b)[l][None, :]
        for h in range(4):
            gmwT[l, :, h * 128:(h + 1) * 128] = gm_w_s[l, h].T
        for g in range(4):
            c, q = g // 2, g % 2
            pbd[l, c * 128 + q * 64:c * 128 + (q + 1) * 64, q * 64:(q + 1) * 64] = pool_w[l, g]
    rbb = np.ascontiguousarray(np.broadcast_to(f(router_b)[None, :], (128, NE))).astype(np.float32)
    shared = {
        "pp": pp, "bc3": bc3, "lnp": lnp, "gmwT": gmwT, "pbd": pbd, "rbb": rbb,
        "w_in": f(w_in), "w_o": f(w_o), "cf_pw": f(cf_pw), "router_w": f(router_w),
        "exp_w_gate": f(exp_w_gate), "exp_w_up": f(exp_w_up), "exp_w_down": f(exp_w_down),
    }
    in_maps = []
    for c in range(NCORES):
        bi, half = c // 2, c % 2
        xc = np.zeros((T, D), np.float32)
        xc[HALO:] = x[bi, half * TM:(half + 1) * TM]
        if half == 1:
            xc[:HALO] = x[bi, TM - HALO:TM]
        ident, maskT, rcfix, invw, hflag = _host_consts(half)
        m = dict(shared)
        m.update({"x": xc, "ident": ident, "maskT": maskT, "rcfix": rcfix, "invw": invw, "hflag": hflag})
        in_maps.append(m)
    if "nc" not in _CACHE:
        _CACHE["nc"] = build()
    res = run_bass_kernel_spmd(_CACHE["nc"], in_maps, core_ids=list(range(NCORES)))
    out = np.zeros((4, SEQ, D), np.float32)
    for c in range(NCORES):
        bi, half = c // 2, c % 2
        out[bi, half * TM:(half + 1) * TM] = res.results[c]["out"]
    return out
```

```python
from contextlib import ExitStack
import numpy as np
import concourse.bass as bass
import concourse.mybir as mybir
from concourse.bass_utils import run_bass_kernel_spmd

F32 = mybir.dt.float32
BF16 = mybir.dt.bfloat16
ALU = mybir.AluOpType
AF = mybir.ActivationFunctionType
AX = mybir.AxisListType
_DSZ = {F32: 4, BF16: 2}

L = 2
D = 1024
NCORES = 8
SEQ = 4096
TM = 2048
HALO = 128
T = TM + HALO
NT = T // 128
NE = 16
DE = 512
ALPHA = float((2 * L) ** 0.25)
LN_EPS = 1e-5
RMS_EPS = 1e-6
NB = 512
XNB = 512
XBLOCKS = [(0, 128)] + [(128 + i * XNB, XNB) for i in range(TM // XNB)]
MBLOCKS = [(0, 128)] + [(128 + i * NB, NB) for i in range(TM // NB)]
XOFF = []
_o = 0
for (_t0, _nb) in XBLOCKS:
    XOFF.append(_o)
    _o += 8 * _nb
XT_COLS = _o
NPP = 86
PP_DW = 0
PP_DWB = 62
PP_CLG = 64
PP_CLB = 66
PP_PSC = 68
PP_SCW = 70
PP_MG = 76
PP_BS = 82


class Ref:
    __slots__ = ("ap", "buf", "box")

    def __init__(self, ap, buf, box):
        self.ap = ap
        self.buf = buf
        self.box = box

    def v(self, fn):
        return Ref(fn(self.ap), self.buf, self.box)


class Buf:
    def __init__(self, handle, name, P, F, dtype):
        self.h = handle
        self.name = name
        self.P = P
        self.F = F
        self.dtype = dtype
        self.esz = _DSZ[dtype]
        self.entries = {}
        self.whole = False

    def __call__(self, f0=0, f1=None, p0=0, p1=None):
        if f1 is None:
            f1 = self.F
        if p1 is None:
            p1 = self.P
        assert 0 <= f0 < f1 <= self.F and 0 <= p0 < p1 <= self.P, (self.name, f0, f1, p0, p1)
        if self.whole:
            return Ref(self.h[p0:p1, f0:f1], self, (0, self.P, 0, self.F * self.esz))
        return Ref(self.h[p0:p1, f0:f1], self, (p0, p1, f0 * self.esz, f1 * self.esz))


def _ov(a, b):
    return a[0] < b[1] and b[0] < a[1] and a[2] < b[3] and b[2] < a[3]


def _cov(o, i):
    return o[0] <= i[0] and i[1] <= o[1] and o[2] <= i[2] and i[3] <= o[3]


class Eng:
    def __init__(self, name, eng):
        self.name = name
        self.e = eng
        self.sem = name
        self.ticket = 0
        self.seen = {}


class Emitter:
    def __init__(self, nc, sems):
        self.nc = nc
        self.sems = sems
        self.pe = Eng("pe", nc.tensor)
        self.act = Eng("act", nc.scalar)
        self.dve = Eng("dve", nc.vector)
        self.pool = Eng("pool", nc.gpsimd)
        self.sp = Eng("sp", nc.sync)
        self.dma_count = {}
        self.n_inst = 0
        self.n_wait = 0
        self.rec = None

    def _collect(self, E, reads, writes, is_dma):
        need = {}
        for r in reads:
            rb = r.box
            for (box, sk, w), val in r.buf.entries.items():
                if w and _ov(box, rb) and need.get(sk, 0) < val:
                    need[sk] = val
        for wr in writes:
            wb = wr.box
            for (box, sk, w), val in wr.buf.entries.items():
                if _ov(box, wb):
                    if sk == E.sem and E.name == "pe" and not is_dma:
                        continue
                    if need.get(sk, 0) < val:
                        need[sk] = val
        return need

    def _waits(self, E, need):
        for sk, val in need.items():
            if E.seen.get(sk, 0) >= val:
                continue
            E.e.wait_ge(self.sems[sk], val)
            E.seen[sk] = val
            self.n_wait += 1

    def _record(self, reads, writes, sk, val):
        for wr in writes:
            ent = wr.buf.entries
            wb = wr.box
            dead = [k for k in ent if _cov(wb, k[0])]
            for k in dead:
                del ent[k]
            ent[(wb, sk, True)] = val
        for r in reads:
            r.buf.entries[(r.box, sk, False)] = val

    def replay(self, rec):
        self.op(rec[0], rec[1], rec[2], rec[3])

    def op(self, E, fn, w=(), r=()):
        if self.rec is not None:
            self.rec.append((E, fn, tuple(w), tuple(r)))
            return None
        self._waits(E, self._collect(E, r, w, False))
        ins = fn(E.e)
        E.ticket += 1
        ins.then_inc(self.sems[E.sem], 1)
        self._record(r, w, E.sem, E.ticket)
        self.n_inst += 1
        return ins

    def dma(self, E, out, in_, semkey, w=(), r=(), group_left=0):
        self._waits(E, self._collect(E, r, w, True))
        ins = E.e.dma_start(out=out, in_=in_)
        c = self.dma_count.get(semkey, 0) + 16
        self.dma_count[semkey] = c
        ins.then_inc(self.sems[semkey], 16)
        self._record(r, w, semkey, c + 16 * group_left)
        self.n_inst += 1
        return ins


def build(n_layers=L, stop=None):
    nc = bass.Bass("TRN2", target_bir_lowering=False)

    def din(name, shape):
        return nc.dram_tensor(name, list(shape), F32, kind="ExternalInput").ap()

    x_d = din("x", [T, D])
    ident_d = din("ident", [128, 128])
    maskT_d = din("maskT", [128, 128])
    rcfix_d = din("rcfix", [128, 32])
    invw_d = din("invw", [128, 2])
    hflag_d = din("hflag", [128, 1])
    rbb_d = din("rbb", [128, NE])
    pp_d = din("pp", [L, 128, NPP])
    bc3_d = din("bc3", [L, 128, 768])
    lnp_d = din("lnp", [L, 128, 4096])
    gmwT_d = din("gmwT", [L, 128, 512])
    pbd_d = din("pbd", [L, 256, 128])
    w_in_d = din("w_in", [L, D, 2048])
    w_o_d = din("w_o", [L, D, D])
    cf_pw_d = din("cf_pw", [L, 256, 256])
    rw_d = din("router_w", [D, NE])
    wg_d = din("exp_w_gate", [L, NE, D, DE])
    wu_d = din("exp_w_up", [L, NE, D, DE])
    wd_d = din("exp_w_down", [L, NE, DE, D])
    out_d = nc.dram_tensor("out", [TM, D], F32, kind="ExternalOutput").ap()

    with ExitStack() as st:
        def sb(name, F, dt=F32, P=128):
            h = st.enter_context(nc.sbuf_tensor("s_" + name, [P, F], dt))
            return Buf(h, name, P, F, dt)

        def ps(name, F, dt=F32):
            h = st.enter_context(nc.psum_tensor(name, [128, F], dt))
            bf = Buf(h, name, 128, F, dt)
            bf.whole = True
            return bf

        semnames = ["pe", "act", "dve", "pool", "sp", "dx", "dc", "dpar", "dwin", "dwo",
                    "dwg0", "dwu0", "dwd0", "dwg1", "dwu1", "dwd1", "dout", "dln2", "dcp", "dparp"]
        sems = {k: st.enter_context(nc.semaphore(k)) for k in semnames}
        em = Emitter(nc, sems)
        PE, ACT, DVE, POOL, SP = em.pe, em.act, em.dve, em.pool, em.sp

        xtok = sb("xtok", NT * D)
        xT = sb("xT", XT_COLS, BF16)
        wbuf = sb("wbuf", 24576, BF16)
        lnp = sb("lnp", 2048)
        bc3 = sb("bc3", 768)
        ppb = sb("ppb", NPP)
        identf = sb("identf", 128)
        identb = sb("identb", 128, BF16)
        maskT = sb("maskT", 128)
        ones = sb("ones", 128)
        rcfix = sb("rcfix", 32)
        invw = sb("invw", 2)
        hflag = sb("hflag", 1)
        rbb = sb("rbb", NE)
        wmT = sb("wmT", 512, BF16)
        pwb = sb("pwb", 512, BF16)
        pbd = sb("pbd", 256, BF16)
        rwb = sb("rwb", 8 * NE, BF16)
        gtmp = sb("gtmp", 512)

        def gates(a=0, c=NT * NE):
            return gtmp(a, c)
        yT = sb("yT", 8 * NB, BF16)
        SW = NB + 32
        NS = 9
        ZL0, CH0, TM0 = 0, 2, 4
        scr = sb("scr", NS * SW)
        scrb = sb("scrb", 2 * 4 * XNB, BF16)
        HBW = 30 + NB + 2
        hbb = sb("hbb", 2 * HBW, BF16)
        dg = sb("dg", 4 * 128, BF16)
        sm = sb("sm", 384)

        def rt(a, c):
            return scr(2 * SW + a, 2 * SW + c)

        def SG(k, nb):
            return scr(k * SW, k * SW + nb)

        xbs = [(lambda a=0, c=D, k=k: scrb(2048 + k * D + a, 2048 + k * D + c)) for k in range(2)]

        def wmS(a=0, c=512):
            return scr(7 * SW + a, 7 * SW + c)

        ACC = [lambda a, c, p0=0, p1=128, k=k: scr((7 + k) * SW + a, (7 + k) * SW + c, p0, p1) for k in range(2)]

        def S(i, f0=0, f1=SW, p0=0, p1=128):
            return scr(i * SW + f0, i * SW + f1, p0, p1)

        B = [ps("pb%d" % i, 512) for i in range(6)]
        pTs = [ps("pT%d" % i, 1024, BF16) for i in range(2)]

        def mm(out, lhsT, rhs, start, stop):
            em.op(PE, lambda e: e.matmul(out.ap, lhsT.ap, rhs.ap, start=start, stop=stop), w=[out], r=[lhsT, rhs])

        def act(out, in_, func, bias=None, scale=None, extra_r=()):
            kw = {}
            if bias is not None:
                kw["bias"] = bias.ap if isinstance(bias, Ref) else bias
            if scale is not None:
                kw["scale"] = scale.ap if isinstance(scale, Ref) else scale
            rr = [in_] + [a for a in (bias, scale) if isinstance(a, Ref)] + list(extra_r)
            em.op(ACT, lambda e: e.activation(out=out.ap, in_=in_.ap, func=func, **kw), w=[out], r=rr)

        def tt(E, out, a, b, op):
            em.op(E, lambda e: e.tensor_tensor(out=out.ap, in0=a.ap, in1=b.ap, op=op), w=[out], r=[a, b])

        def ts(E, out, a, s1, op0, s2=None, op1=None):
            rr = [a] + [s for s in (s1, s2) if isinstance(s, Ref)]
            a1 = s1.ap if isinstance(s1, Ref) else s1
            a2 = s2.ap if isinstance(s2, Ref) else s2
            if op1 is None:
                em.op(E, lambda e: e.tensor_scalar(out=out.ap, in0=a.ap, scalar1=a1, scalar2=None, op0=op0), w=[out], r=rr)
            else:
                em.op(E, lambda e: e.tensor_scalar(out=out.ap, in0=a.ap, scalar1=a1, scalar2=a2, op0=op0, op1=op1),
                      w=[out], r=rr)

        def stt(E, out, a, s, b, op0, op1):
            rr = [a, b] + ([s] if isinstance(s, Ref) else [])
            sv = s.ap if isinstance(s, Ref) else s
            em.op(E, lambda e: e.scalar_tensor_tensor(out=out.ap, in0=a.ap, scalar=sv, in1=b.ap, op0=op0, op1=op1),
                  w=[out], r=rr)

        def cpy(E, out, in_):
            if E is ACT:
                em.op(E, lambda e: e.copy(out=out.ap, in_=in_.ap), w=[out], r=[in_])
            else:
                em.op(E, lambda e: e.tensor_copy(out=out.ap, in_=in_.ap), w=[out], r=[in_])

        def powm(out, in_, n):
            act(out, in_, AF.Ln)
            act(out, out, AF.Exp, scale=-0.5)

        def xloc(t0):
            if t0 < 128:
                return 0, t0
            return 1 + (t0 - 128) // XNB, (t0 - 128) % XNB

        def xT_tok(t0, n, kc):
            xb_, c0 = xloc(t0)
            nbx = XBLOCKS[xb_][1]
            assert c0 + n <= nbx
            return xT(XOFF[xb_] + kc * nbx + c0, XOFF[xb_] + kc * nbx + c0 + n)

        def xtile(i, f0=0, f1=D):
            return xtok(i * D + f0, i * D + f1)

        def pipeline(items, stages):
            n, m = len(items), len(stages)
            for step in range(n + m - 1):
                for k in range(m - 1, -1, -1):
                    idx = step - k
                    if 0 <= idx < n:
                        stages[k](idx, items[idx])

        def SMT(i, a, c):
            return sm(i * 20 + a, i * 20 + c)

        def ln_stages(goff, ring0=0):
            def sA(r, i):
                for c in range(2):
                    em.op(DVE, lambda e, c=c: e.bn_stats(out=SMT(i, c * 6, c * 6 + 6).ap, in_=xtile(i, c * 512, (c + 1) * 512).ap),
                          w=[SMT(i, c * 6, c * 6 + 6)], r=[xtile(i, c * 512, (c + 1) * 512)])
                em.op(DVE, lambda e: e.bn_aggr(out=SMT(i, 12, 14).ap, in_=SMT(i, 0, 12).ap), w=[SMT(i, 12, 14)],
                      r=[SMT(i, 0, 12)])
                ts(DVE, SMT(i, 14, 15), SMT(i, 13, 14), LN_EPS, ALU.add)

            def sB(r, i):
                powm(SMT(i, 15, 16), SMT(i, 14, 15), 1)
                stt(DVE, SMT(i, 16, 17), SMT(i, 12, 13), -1.0, SMT(i, 15, 16), ALU.mult, ALU.mult)

            def sC(r, i):
                act(xtile(i), xtile(i), AF.Identity, bias=SMT(i, 16, 17), scale=SMT(i, 15, 16))

            def sD(r, i):
                tt(POOL, xtile(i), xtile(i), lnp(goff, goff + D), ALU.mult)
                tt(POOL, xtile(i), xtile(i), lnp(goff + D, goff + 2 * D), ALU.add)
            return [sA, sB, sC, sD]

        def xT_stages(scale0=None, alpha_after=False, pt_fixed=None):
            def sE(r, i):
                xb = xbs[r % 2]
                if scale0 is not None and i == 0:
                    act(xb(), xtile(i), AF.Identity, scale=scale0)
                else:
                    cpy(ACT, xb(), xtile(i))
                if alpha_after:
                    em.op(ACT, lambda e: e.mul(out=xtile(i).ap, in_=xtile(i).ap, mul=ALPHA), w=[xtile(i)], r=[xtile(i)])

            def sF(r, i):
                xb = xbs[r % 2]
                pT = pTs[r % 2 if pt_fixed is None else pt_fixed]
                for kc in range(8):
                    em.op(PE, lambda e, kc=kc: e.transpose(out=pT(kc * 128, (kc + 1) * 128).ap, in_=xb(kc * 128, (kc + 1) * 128).ap,
                                                    identity=identb().ap),
                          w=[pT(kc * 128, (kc + 1) * 128)], r=[xb(kc * 128, (kc + 1) * 128), identb()])

            def sG(r, i):
                pT = pTs[r % 2 if pt_fixed is None else pt_fixed]
                b, c0 = xloc(i * 128)
                nb = XBLOCKS[b][1]
                dst = xT(XOFF[b], XOFF[b] + 8 * nb).v(
                    lambda ap: ap.rearrange("p (k t) -> p k t", k=8)[:, :, c0:c0 + 128])
                src = pT().v(lambda ap: ap.rearrange("p (k t) -> p k t", k=8))
                cpy(ACT, dst, src)
            return [sE, sF, sG]

        def rms_group(yf, nb, gcol, c0):
            sq = S(TM0 + 2, 0, nb)
            for j in range(2):
                act(sq, yf[j], AF.Square)
                mm(B[4](0, nb), ones(), sq, j == 0, j == 1)
            ve = S(TM0 + 1, 0, nb)
            ts(DVE, ve, B[4](0, nb), 1.0 / 256.0, ALU.mult, RMS_EPS, ALU.add)
            powm(ve, ve, nb)
            for j in range(2):
                stt(DVE, yT((c0 + j) * nb, (c0 + j + 1) * nb), yf[j], ppb(gcol + j, gcol + j + 1), ve, ALU.mult, ALU.mult)

        for i in range(NT):
            em.dma(SP, xtile(i).ap, x_d[i * 128:(i + 1) * 128, :], "dx", w=[xtile(i)], group_left=NT - 1 - i)
        cl = [(identf, ident_d), (maskT, maskT_d), (rcfix, rcfix_d), (invw, invw_d), (hflag, hflag_d), (rbb, rbb_d)]
        for n, (bf, d) in enumerate(cl):
            em.dma(SP, bf().ap, d, "dc", w=[bf()], group_left=len(cl) - 1 - n)
        em.dma(POOL, rwb().ap.rearrange("p (k n) -> p k n", k=8), rw_d.rearrange("(k p) n -> p k n", p=128), "dcp",
               w=[rwb()])
        cpy(ACT, identb(), identf())
        em.op(DVE, lambda e: e.memset(ones().ap, 1.0), w=[ones()])

        def load_layer_weights(l):
            for q in range(4):
                em.dma(POOL, wbuf(q * 4096, (q + 1) * 4096).ap.rearrange("p (k n) -> p k n", k=2),
                       w_in_d[l, q * 256:(q + 1) * 256, :].rearrange("(k p) n -> p k n", p=128), "dwin",
                       w=[wbuf(q * 4096, (q + 1) * 4096)], group_left=3 - q)
            for q in range(2):
                em.dma(POOL, wbuf(16384 + q * 4096, 16384 + (q + 1) * 4096).ap.rearrange("p (k n) -> p k n", k=4),
                       w_o_d[l, q * 512:(q + 1) * 512, :].rearrange("(k p) n -> p k n", p=128), "dwo",
                       w=[wbuf(16384 + q * 4096, 16384 + (q + 1) * 4096)], group_left=1 - q)

        def load_layer_params(l):
            lst = [(SP, ppb(), pp_d[l]), (SP, bc3(), bc3_d[l]), (SP, lnp(), lnp_d[l, :, 0:2048]), (SP, wmS(), gmwT_d[l])]
            n = len(lst)
            for k, (E, rf, d) in enumerate(lst):
                em.dma(E, rf.ap, d, "dpar", w=[rf], group_left=n - 1 - k)
            em.dma(POOL, pwb().ap.rearrange("p (k n) -> p k n", k=2),
                   cf_pw_d[l].rearrange("(k p) n -> p k n", p=128), "dparp", w=[pwb()], group_left=1)
            em.dma(POOL, pbd().ap.rearrange("p (k n) -> p k n", k=2),
                   pbd_d[l].rearrange("(k p) n -> p k n", p=128), "dparp", w=[pbd()], group_left=0)
            tt(DVE, wmT().v(lambda ap: ap.rearrange("p (h t) -> p h t", h=4)),
               wmS().v(lambda ap: ap.rearrange("p (h t) -> p h t", h=4)),
               maskT().v(lambda ap: ap.rearrange("p (o t) -> p o t", o=1).to_broadcast([128, 4, 128])), ALU.mult)

        def load_expert(l, e):
            s = e % 2
            base = s * 12288
            em.dma(POOL, wbuf(base, base + 4096).ap.rearrange("p (k n) -> p k n", k=8),
                   wg_d[l, e].rearrange("(k p) n -> p k n", p=128), "dwg%d" % s, w=[wbuf(base, base + 4096)])
            em.dma(POOL, wbuf(base + 4096, base + 8192).ap.rearrange("p (k n) -> p k n", k=8),
                   wu_d[l, e].rearrange("(k p) n -> p k n", p=128), "dwu%d" % s, w=[wbuf(base + 4096, base + 8192)])
            em.dma(POOL, wbuf(base + 8192, base + 12288).ap.rearrange("p (k n) -> p k n", k=4),
                   wd_d[l, e].rearrange("(k p) n -> p k n", p=128), "dwd%d" % s, w=[wbuf(base + 8192, base + 12288)])

        load_layer_weights(0)
        load_layer_params(0)
        pipeline(list(range(NT)), xT_stages())

        def win(kc, c0, c1):
            return wbuf(kc * 2048 + c0, kc * 2048 + c1)

        def wo(kc, c0, c1):
            return wbuf(16384 + kc * 1024 + c0, 16384 + kc * 1024 + c1)

        zrot = [0]
        dgi = [0]
        pre_e0 = [False]

        def zbank():
            b = B[2 + zrot[0] % 2]
            zrot[0] += 1
            return b

        def zmm(bank, t0, nb, col0):
            for kc in range(8):
                mm(bank(0, nb), win(kc, col0, col0 + 128), xT_tok(t0, nb, kc), kc == 0, kc == 7)

        def finish_dump():
            for i in range(1, NT):
                em.dma(SP, out_d[(i - 1) * 128:i * 128, :], xtile(i).ap, "dout", r=[xtile(i)])
            SP.e.wait_ge(sems["dout"], em.dma_count["dout"])
            build.stats = (em.n_inst, em.n_wait)

        if stop == "setup":
            finish_dump()
            return nc
        for l in range(n_layers):
            for j in range(2):
                em.op(DVE, lambda e: e.memset(hbb(j * HBW, j * HBW + 30).ap, 0.0), w=[hbb(j * HBW, j * HBW + 30)])
                em.op(DVE, lambda e: e.memset(S(ZL0 + j, 0, 15).ap, 0.0), w=[S(ZL0 + j, 0, 15)])
                em.op(DVE, lambda e: e.memset(S(CH0 + j, 0, 2).ap, 0.0), w=[S(CH0 + j, 0, 2)])

            last = (l == n_layers - 1)
            def G_section(b):
                t0, nb = MBLOCKS[b]
                tiles = list(range(t0 // 128, (t0 + nb) // 128))
                state_only = last and b == 0 and stop is None
                GS = 340

                def g_bufs(r):
                    p = r % 2
                    p = 0
                    pUV, pS_ = B[0], B[1]
                    sl = [(lambda a, c: gtmp(a, c)), (lambda a, c: gtmp(256 + a, 256 + c)), (lambda a, c: gtmp(a, c))]
                    u_sb, vn, yg = sl[0](0, 256), sl[1](0, 256), sl[2](0, 256)
                    q = lambda a, c: sm(GS + a, GS + c)
                    vb = lambda a, c: scrb(1024 + a, 1024 + c)
                    return pUV, pS_, sl, u_sb, vn, yg, q, vb, pTs[1]

                def g0(r, i):
                    pUV = g_bufs(r)[0]
                    for kc in range(8):
                        mm(pUV(), xT_tok(i * 128, 128, kc), win(kc, 0, 512), kc == 0, kc == 7)

                def g1(r, i):
                    pUV, pS_, sl, u_sb, vn, yg, q, vb, pT = g_bufs(r)
                    cpy(ACT, u_sb, pUV(0, 256))
                    cpy(ACT, vn, pUV(256, 512))

                def g2(r, i):
                    pUV, pS_, sl, u_sb, vn, yg, q, vb, pT = g_bufs(r)
                    em.op(DVE, lambda e: e.bn_stats(out=q(0, 6).ap, in_=vn.ap), w=[q(0, 6)], r=[vn])
                    em.op(DVE, lambda e: e.bn_aggr(out=q(6, 8).ap, in_=q(0, 6).ap), w=[q(6, 8)], r=[q(0, 6)])
                    ts(DVE, q(8, 9), q(7, 8), LN_EPS, ALU.add)

                def g3(r, i):
                    pUV, pS_, sl, u_sb, vn, yg, q, vb, pT = g_bufs(r)
                    powm(q(9, 10), q(8, 9), 1)

                def g4(r, i):
                    pUV, pS_, sl, u_sb, vn, yg, q, vb, pT = g_bufs(r)
                    ts(DVE, vn, vn, q(6, 7), ALU.subtract, q(9, 10), ALU.mult)
                    tt(POOL, vn, vn, bc3(0, 256), ALU.mult)
                    tt(POOL, vb(0, 256), vn, bc3(256, 512), ALU.add)

                def g5(r, i):
                    pUV, pS_, sl, u_sb, vn, yg, q, vb, pT = g_bufs(r)
                    for h in range(4):
                        mm(pS_(64 * h, 64 * h + 64), wmT(128 * h, 128 * h + 128), vb(64 * h, 64 * h + 64), True, True)

                def g6(r, i):
                    pUV, pS_, sl, u_sb, vn, yg, q, vb, pT = g_bufs(r)
                    for h in range(4):
                        stt(DVE, sl[2](64 * h, 64 * h + 64), pS_(64 * h, 64 * h + 64), ppb(PP_BS + h, PP_BS + h + 1),
                            sl[0](64 * h, 64 * h + 64), ALU.add, ALU.mult)
                    em.op(DVE, lambda e: e.bn_stats(out=q(10, 16).ap, in_=yg.ap), w=[q(10, 16)], r=[yg])
                    em.op(DVE, lambda e: e.bn_aggr(out=q(16, 18).ap, in_=q(10, 16).ap), w=[q(16, 18)], r=[q(10, 16)])
                    stt(DVE, q(18, 19), q(16, 17), q(16, 17), q(17, 18), ALU.mult, ALU.add)
                    ts(DVE, q(18, 19), q(18, 19), RMS_EPS, ALU.add)

                def g7(r, i):
                    pUV, pS_, sl, u_sb, vn, yg, q, vb, pT = g_bufs(r)
                    powm(q(19, 20), q(18, 19), 1)
                    stt(DVE, vb(256, 512), yg, q(19, 20), bc3(512, 768), ALU.mult, ALU.mult)

                def g8(r, i):
                    pUV, pS_, sl, u_sb, vn, yg, q, vb, pT = g_bufs(r)
                    for c in range(2):
                        em.op(PE, lambda e, c=c: e.transpose(out=pT(c * 128, (c + 1) * 128).ap,
                                                        in_=vb(256 + c * 128, 256 + (c + 1) * 128).ap,
                                                        identity=identb().ap),
                              w=[pT(c * 128, (c + 1) * 128)], r=[vb(256 + c * 128, 256 + (c + 1) * 128), identb()])

                def g9(r, i):
                    pT = g_bufs(r)[8]
                    c0 = i * 128 - t0
                    dst = yT(0, 2 * nb).v(lambda ap: ap.rearrange("p (k t) -> p k t", k=2)[:, :, c0:c0 + 128])
                    cpy(ACT, dst, pT(0, 256).v(lambda ap: ap.rearrange("p (k t) -> p k t", k=2)))

                for i_ in tiles:
                    for gs in [g0, g1, g2, g3, g4, g5, g6, g7, g8, g9]:
                        gs(0, i_)


            def CPS_section(b):
                t0, nb = MBLOCKS[b]
                tiles = list(range(t0 // 128, (t0 + nb) // 128))
                state_only = last and b == 0 and stop is None
                accs = []
                for j in range(2):
                    pa = zbank()
                    zmm(pa, t0, nb, 512 + j * 128)
                    pg = zbank()
                    zmm(pg, t0, nb, 768 + j * 128)
                    sig = S(TM0 + j, 0, nb)
                    act(sig, pg(0, nb), AF.Tanh, scale=0.5)
                    hb = lambda a, c, j=j: hbb(j * HBW + a, j * HBW + c)
                    stt(DVE, hb(30, 30 + nb), sig, 1.0, pa(0, nb), ALU.add, ALU.mult)
                hbs = [(lambda a, c, j=j: hbb(j * HBW + a, j * HBW + c)) for j in range(2)]
                if state_only:
                    for j in range(2):
                        cpy(ACT, hbs[j](0, 30), hbs[j](nb, nb + 30))
                else:
                    pcs = [zbank(), zbank()]
                    for k in range(31):
                        for j in range(2):
                            slot = dgi[0] % 4
                            dgi[0] += 1
                            wcol = ppb(PP_DW + j * 31 + k, PP_DW + j * 31 + k + 1)
                            GE = (POOL, ACT, POOL, ACT)[slot]
                            if GE is ACT:
                                act(dg(slot * 128, (slot + 1) * 128), identf(), AF.Identity, scale=wcol)
                            elif GE is DVE:
                                ts(GE, dg(slot * 128, (slot + 1) * 128), identf(), wcol, ALU.mult)
                            else:
                                ts(GE, dg(slot * 128, (slot + 1) * 128), identf(), wcol, ALU.mult, 0.0, ALU.add)
                            mm(pcs[j](0, nb), dg(slot * 128, (slot + 1) * 128), hbs[j](k, k + nb), k == 0, k == 30)
                    for j in range(2):
                        act(ACC[j](0, nb), pcs[j](0, nb), AF.Identity, bias=ppb(PP_DWB + j, PP_DWB + j + 1), scale=0.5)
                    for j in range(2):
                        cpy(ACT, hbs[j](0, 30), hbs[j](nb, nb + 30))
                        accs.append(ACC[j](0, nb))
                if state_only:
                    for j in range(2):
                        pz = zbank()
                        zmm(pz, t0, nb, 1024 + j * 128)
                        cpy(ACT, S(ZL0 + j, 15, 15 + nb), pz(0, nb))
                        cpy(ACT, S(ZL0 + j, 0, 15), S(ZL0 + j, nb, nb + 15))
                        pC_ = zbank()
                        zmm(pC_, t0, nb, 1536 + j * 128)
                        pH_ = zbank()
                        zmm(pH_, t0, nb, 1792 + j * 128)
                        cs = S(TM0, 0, nb)
                        cpy(ACT, cs, pC_(0, nb))
                        tt(DVE, S(CH0 + j, 2, 2 + nb), pH_(0, nb), cs, ALU.mult)
                        cpy(ACT, S(CH0 + j, 0, 2), S(CH0 + j, nb, nb + 2))
                    return
                sq = S(TM0 + 2, 0, nb)
                for j in range(2):
                    act(sq, accs[j], AF.Square)
                    mm(B[5](0, nb), ones(), accs[j], j == 0, j == 1)
                    mm(B[4](0, nb), ones(), sq, j == 0, j == 1)
                mean = S(TM0, 0, nb)
                ts(DVE, mean, B[5](0, nb), 1.0 / 256.0, ALU.mult)
                msq = S(TM0 + 1, 0, nb)
                tt(DVE, msq, mean, mean, ALU.mult)
                ve = S(TM0 + 2, 0, nb)
                ts(DVE, ve, B[4](0, nb), 1.0 / 256.0, ALU.mult, LN_EPS, ALU.add)
                tt(DVE, ve, ve, msq, ALU.subtract)
                powm(ve, ve, nb)
                for j in range(2):
                    tt(DVE, accs[j], accs[j], mean, ALU.subtract)
                    tt(DVE, accs[j], accs[j], ve, ALU.mult)
                    act(scrb(j * nb, (j + 1) * nb), accs[j], AF.Silu, bias=ppb(PP_CLB + j, PP_CLB + j + 1),
                        scale=ppb(PP_CLG + j, PP_CLG + j + 1))
                yf = []
                for jo in range(2):
                    for j in range(2):
                        mm(B[5](0, nb), pwb(j * 256 + jo * 128, j * 256 + (jo + 1) * 128), scrb(j * nb, (j + 1) * nb),
                           j == 0, j == 1)
                    cpy(ACT, ACC[jo](0, nb), B[5](0, nb))
                    yf.append(ACC[jo](0, nb))
                rms_group(yf, nb, PP_MG + 0, 2)

                zls = [(lambda a, c, p0=0, p1=128, j=j: S(ZL0 + j, a, c, p0, p1)) for j in range(2)]
                Xs = [(lambda a, c, p0=0, p1=128: S(TM0, a, c, p0, p1)), (lambda a, c, p0=0, p1=128: S(TM0 + 2, a, c, p0, p1))]
                Ys = [(lambda a, c, p0=0, p1=128: S(TM0 + 1, a, c, p0, p1)), ACC[1]]
                pls = [(lambda a, c, p0=0, p1=128, j=j: scrb(j * nb + a, j * nb + c, p0, p1)) for j in range(2)]
                pzb = [B[2], B[3]]
                pob = [B[5], B[4]]
                for j in range(2):
                    zmm(pzb[j], t0, nb, 1024 + j * 128)
                for j in range(2):
                    cpy(ACT, zls[j](15, 15 + nb), pzb[j](0, nb))
                for j in range(2):
                    tt(DVE, Xs[j](1, 15 + nb), zls[j](1, 15 + nb), zls[j](0, 14 + nb), ALU.add)
                for j in range(2):
                    tt(DVE, Ys[j](3, 15 + nb), Xs[j](3, 15 + nb), Xs[j](1, 13 + nb), ALU.add)
                tt(DVE, Xs[1](7, 15 + nb), Ys[1](7, 15 + nb), Ys[1](3, 11 + nb), ALU.add)
                tt(DVE, Ys[1](15, 15 + nb), Xs[1](15, 15 + nb), Xs[1](7, 7 + nb), ALU.add)
                for (p0, p1) in ((0, 64), (64, 128)):
                    for j in range(2):
                        src = Xs[j] if p0 == 0 else Ys[j]
                        stt(DVE, pls[j](0, nb, p0, p1), src(15, 15 + nb, p0, p1), invw(j, j + 1, p0, p1),
                            zls[j](15, 15 + nb, p0, p1), ALU.mult, ALU.subtract)
                if b == 1:
                    for j in range(2):
                        for (p0, p1) in ((0, 64), (64, 128)):
                            src = Xs[j] if p0 == 0 else Ys[j]
                            tmp = sm(368, 384, p0, p1)
                            tt(DVE, tmp, src(15, 31, p0, p1), rcfix(j * 16, j * 16 + 16, p0, p1), ALU.mult)
                            tt(DVE, pls[j](0, 16, p0, p1), tmp, zls[j](15, 31, p0, p1), ALU.subtract)
                for j in range(2):
                    cpy(ACT, zls[j](0, 15), zls[j](nb, nb + 15))
                for j in range(2):
                    mm(pob[j](0, nb), pbd(j * 128, (j + 1) * 128), pls[j](0, nb), True, True)
                for j in range(2):
                    act(ACC[j](0, nb), pob[j](0, nb), AF.Identity, scale=ppb(PP_PSC + j, PP_PSC + j + 1))
                rms_group([ACC[0](0, nb), ACC[1](0, nb)], nb, PP_MG + 2, 4)

                def zmm_to(bank, col0):
                    zmm(bank, t0, nb, col0)
                    return bank
                sbank = [(B[2], B[3], B[2]), (B[4], B[5], B[4])]
                chls = [(lambda a, c, j=j: S(CH0 + j, a, c)) for j in range(2)]
                css = [S(TM0 + j, 0, nb) for j in range(2)]
                for j in range(2):
                    zmm_to(sbank[j][0], 1536 + j * 128)
                    zmm_to(sbank[j][1], 1792 + j * 128)
                for j in range(2):
                    cpy(ACT, css[j], sbank[j][0](0, nb))
                for j in range(2):
                    tt(DVE, chls[j](2, 2 + nb), sbank[j][1](0, nb), css[j], ALU.mult)
                for j in range(2):
                    zmm_to(sbank[j][2], 1280 + j * 128)
                for j in range(2):
                    ts(DVE, ACC[j](0, nb), chls[j](0, nb), ppb(PP_SCW + j * 3, PP_SCW + j * 3 + 1), ALU.mult)
                for k in (1, 2):
                    for j in range(2):
                        stt(DVE, ACC[j](0, nb), chls[j](k, k + nb), ppb(PP_SCW + j * 3 + k, PP_SCW + j * 3 + k + 1),
                            ACC[j](0, nb), ALU.mult, ALU.add)
                for j in range(2):
                    tt(DVE, ACC[j](0, nb), sbank[j][2](0, nb), ACC[j](0, nb), ALU.mult)
                for j in range(2):
                    cpy(ACT, chls[j](0, 2), chls[j](nb, nb + 2))
                rms_group([ACC[0](0, nb), ACC[1](0, nb)], nb, PP_MG + 4, 6)


            def O0_section(b):
                t0, nb = MBLOCKS[b]
                tiles = list(range(t0 // 128, (t0 + nb) // 128))
                for r, i in enumerate(tiles):
                    c0 = i * 128 - t0
                    banks = (B[0], B[1]) if r % 2 == 0 else (B[2], B[3])
                    for nh in range(2):
                        for kc in range(8):
                            mm(banks[nh](), yT(kc * nb + c0, kc * nb + c0 + 128), wo(kc, nh * 512, (nh + 1) * 512),
                               kc == 0, kc == 7)
                        stt(DVE, xtile(i, nh * 512, (nh + 1) * 512), xtile(i, nh * 512, (nh + 1) * 512), ALPHA, banks[nh](),
                            ALU.mult, ALU.add)

            def A_section(b):
                t0, nb = MBLOCKS[b]
                tiles = list(range(t0 // 128, (t0 + nb) // 128))
                pipeline(tiles, ln_stages(0) + xT_stages(alpha_after=True, pt_fixed=0))

            def recorded(fn, *a):
                em.rec = []
                fn(*a)
                r_, em.rec = em.rec, None
                return r_

            def interleave(ra, rb):
                na, nb_ = len(ra), len(rb)
                out, ia, ib = [], 0, 0
                while ia < na or ib < nb_:
                    if ib >= nb_ or (ia < na and ia * nb_ <= ib * na):
                        out.append(ra[ia]); ia += 1
                    else:
                        out.append(rb[ib]); ib += 1
                return out

            nblk = len(MBLOCKS)
            skip0 = last and stop is None
            if skip0:
                CPS_section(0)
            fb = 1 if skip0 else 0
            G_section(fb)
            CPS_section(fb)
            O0_section(fb)
            for b in range(fb, nblk):
                ra = recorded(A_section, b)
                if b + 1 < nblk:
                    x = interleave(ra, recorded(G_section, b + 1))
                    for rec in interleave(x, recorded(CPS_section, b + 1)):
                        em.replay(rec)
                    O0_section(b + 1)
                    pass
                else:
                    for rec in ra:
                        em.replay(rec)
            if stop == "mixer":
                finish_dump()
                return nc
            em.dma(SP, lnp().ap, lnp_d[l, :, 2048:4096], "dln2", w=[lnp()])
            if not pre_e0[0]:
                load_expert(l, 0)
            pre_e0[0] = False
            load_expert(l, 1)
            NL = NT * NE
            for i in range(NT):
                for kc in range(8):
                    mm(B[4](i * NE, (i + 1) * NE), xT_tok(i * 128, 128, kc), rwb(kc * NE, (kc + 1) * NE), kc == 0, kc == 7)
            lg = rt(0, NL)
            sel = rt(NL, 2 * NL)
            msk = rt(2 * NL, 3 * NL)
            eq1 = rt(3 * NL, 4 * NL)
            eq2 = rt(4 * NL, 5 * NL)
            tmp = rt(5 * NL, 6 * NL)
            pr = rt(3 * NL, 3 * NL + 68 * 6)
            o = 6 * NL
            gs = rt(o, o + 68)
            gmax = rt(o + 68, o + 85)
            m1 = rt(o + 85, o + 102)
            la = rt(o + 102, o + 119)
            lb = rt(o + 119, o + 136)
            g1 = rt(o + 136, o + 153)
            g2 = rt(o + 153, o + 170)
            v4 = lambda rf: rf.v(lambda ap: ap.rearrange("p (g f) -> p g f", f=4))
            v16 = lambda rf: rf.v(lambda ap: ap.rearrange("p (t e) -> p t e", e=NE))
            bc16 = lambda rf: rf.v(lambda ap: ap.rearrange("p (t o) -> p t o", o=1).to_broadcast([128, NT, NE]))
            cpy(DVE, lg, B[4](0, NL))
            tt(DVE, v16(sel), v16(lg),
               rbb().v(lambda ap: ap.rearrange("p (o e) -> p o e", o=1).to_broadcast([128, NT, NE])), ALU.add)
            s4v = sel.v(lambda ap: ap.rearrange("p (g f) -> p g f", f=4))
            p6 = lambda a, c: pr.v(lambda ap: ap.rearrange("p (g f) -> p g f", f=6)[:, :, a:c])
            s4s = lambda a, c: sel.v(lambda ap: ap.rearrange("p (g f) -> p g f", f=4)[:, :, a:c])
            tt(DVE, p6(0, 3), s4s(0, 3), s4s(1, 4), ALU.add)
            tt(DVE, p6(3, 5), s4s(0, 2), s4s(2, 4), ALU.add)
            tt(DVE, p6(5, 6), s4s(0, 1), s4s(3, 4), ALU.add)
            em.op(DVE, lambda e: e.reduce_max(out=gs.ap, in_=pr.ap.rearrange("p (g f) -> p g f", f=6), axis=AX.X),
                  w=[gs], r=[pr])
            em.op(DVE, lambda e: e.reduce_max(out=gmax.ap, in_=gs.ap.rearrange("p (t g) -> p t g", g=4), axis=AX.X),
                  w=[gmax], r=[gs])
            ing = rt(o + 170, o + 238)
            tt(DVE, ing.v(lambda ap: ap.rearrange("p (t g) -> p t g", g=4)),
               gs.v(lambda ap: ap.rearrange("p (t g) -> p t g", g=4)),
               gmax.v(lambda ap: ap.rearrange("p (t o) -> p t o", o=1).to_broadcast([128, NT, 4])), ALU.is_equal)
            ts(DVE, ing, ing, 1.0, ALU.subtract, 1e30, ALU.mult)
            tt(DVE, v4(msk), v4(sel),
               ing.v(lambda ap: ap.rearrange("p (g o) -> p g o", o=1).to_broadcast([128, 68, 4])), ALU.add)
            em.op(DVE, lambda e: e.reduce_max(out=m1.ap, in_=msk.ap.rearrange("p (t e) -> p t e", e=NE), axis=AX.X),
                  w=[m1], r=[msk])
            tt(DVE, v16(eq1), v16(msk), bc16(m1), ALU.is_equal)
            stt(DVE, msk, eq1, -1e30, msk, ALU.mult, ALU.add)
            em.op(DVE, lambda e: e.reduce_max(out=m1.ap, in_=msk.ap.rearrange("p (t e) -> p t e", e=NE), axis=AX.X),
                  w=[m1], r=[msk])
            tt(DVE, v16(eq2), v16(msk), bc16(m1), ALU.is_equal)
            tt(DVE, tmp, eq1, lg, ALU.mult)
            em.op(DVE, lambda e: e.reduce_sum(out=la.ap, in_=tmp.ap.rearrange("p (t e) -> p t e", e=NE), axis=AX.X),
                  w=[la], r=[tmp])
            tt(DVE, tmp, eq2, lg, ALU.mult)
            em.op(DVE, lambda e: e.reduce_sum(out=lb.ap, in_=tmp.ap.rearrange("p (t e) -> p t e", e=NE), axis=AX.X),
                  w=[lb], r=[tmp])
            tt(DVE, la, la, lb, ALU.subtract)
            act(g1, la, AF.Sigmoid)
            act(g2, la, AF.Sigmoid, scale=-1.0)
            tt(DVE, v16(eq1), v16(eq1), bc16(g1), ALU.mult)
            tt(DVE, v16(eq2), v16(eq2), bc16(g2), ALU.mult)
            tt(DVE, gates(), eq1, eq2, ALU.add)
            if stop == "router":
                finish_dump()
                return nc

            def ln2_block(tl):
                if l + 1 < n_layers:
                    pipeline(tl, ln_stages(0) + xT_stages(scale0=hflag()))
                else:
                    def sOut(r, i):
                        if i >= 1:
                            em.dma(SP, out_d[(i - 1) * 128:i * 128, :], xtile(i).ap, "dout", r=[xtile(i)])
                    pipeline(tl, ln_stages(0) + [sOut])

            pend_ln = []
            ln2_st = ln_stages(0)

            def ln2_out(r, i):
                if i >= 1:
                    em.dma(SP, out_d[(i - 1) * 128:i * 128, :], xtile(i).ap, "dout", r=[xtile(i)])

            it = 0
            for e_ in range(NE):
                spread_ln = (e_ == NE - 1) and stop is None
                s = e_ % 2
                base = s * 12288
                for b, (t0, nb) in enumerate(XBLOCKS):
                    if last and b == 0 and stop is None:
                        continue
                    tiles = list(range(t0 // 128, (t0 + nb) // 128))
                    hb0 = (it % 2) * 4 * XNB
                    for fc in range(4):
                        pG = B[(fc % 2) * 2]
                        pU = B[(fc % 2) * 2 + 1]
                        for kc in range(8):
                            mm(pG(0, nb), wbuf(base + kc * 512 + fc * 128, base + kc * 512 + (fc + 1) * 128),
                               xT_tok(t0, nb, kc), kc == 0, kc == 7)
                        for kc in range(8):
                            mm(pU(0, nb), wbuf(base + 4096 + kc * 512 + fc * 128, base + 4096 + kc * 512 + (fc + 1) * 128),
                               xT_tok(t0, nb, kc), kc == 0, kc == 7)
                        sg = SG(fc % 2, nb)
                        act(sg, pG(0, nb), AF.Silu)
                        tt(DVE, scrb(hb0 + fc * nb, hb0 + (fc + 1) * nb), sg, pU(0, nb), ALU.mult)
                        if spread_ln and pend_ln:
                            for r_, i_ in enumerate(pend_ln[0]):
                                ln2_st[fc](r_, i_)
                    if spread_ln and pend_ln:
                        for r_, i_ in enumerate(pend_ln.pop()):
                            if last:
                                ln2_out(r_, i_)
                    for i in tiles:
                        c0 = i * 128 - t0
                        for nh in range(2):
                            pY = B[4 + nh]
                            for fc in range(4):
                                mm(pY(), scrb(hb0 + fc * nb + c0, hb0 + fc * nb + c0 + 128),
                                   wbuf(base + 8192 + fc * 1024 + nh * 512, base + 8192 + fc * 1024 + (nh + 1) * 512),
                                   fc == 0, fc == 3)
                            stt(DVE, xtile(i, nh * 512, (nh + 1) * 512), pY(), gates(i * NE + e_, i * NE + e_ + 1),
                                xtile(i, nh * 512, (nh + 1) * 512), ALU.mult, ALU.add)
                    it += 1
                    if spread_ln:
                        pend_ln.append(tiles)
                if stop == "e%d" % e_:
                    finish_dump()
                    return nc
                if e_ + 2 < NE:
                    load_expert(l, e_ + 2)
            if l + 1 < n_layers:
                load_layer_weights(l + 1)
            while pend_ln:
                tl = pend_ln.pop()
                if last:
                    ln2_block(tl)
                else:
                    pipeline(tl, ln_stages(0))
            if stop is None and not last:
                pipeline(list(range(NT)), xT_stages(scale0=hflag()))
            if stop is not None:
                ln2_block([i for i in range(NT)])
            if l + 1 < n_layers:
                load_layer_params(l + 1)

        SP.e.wait_ge(sems["dout"], em.dma_count["dout"])
        build.stats = (em.n_inst, em.n_wait)
    return nc


_CACHE = {}


def _host_consts(half):
    ident = np.eye(128, dtype=np.float32)
    s = np.arange(128)
    maskT = (s[:, None] <= s[None, :]).astype(np.float32)
    wins = np.array([2, 4, 8, 16])
    p = np.arange(128)
    rcfix = np.zeros((128, 32), np.float32)
    invw = np.zeros((128, 2), np.float32)
    for j in range(2):
        w = wins[2 * j + (p // 64)].astype(np.float32)
        invw[:, j] = 1.0 / w
        t = np.arange(16, dtype=np.float32)[None, :]
        if half == 0:
            rcfix[:, j * 16:(j + 1) * 16] = 1.0 / np.minimum(t + 1.0, w[:, None])
        else:
            rcfix[:, j * 16:(j + 1) * 16] = 1.0 / w[:, None]
    hflag = np.full((128, 1), float(half), np.float32)
    return ident, maskT, rcfix, invw, hflag


def kernel(x, w_in, gm_ln_g, gm_ln_b, gm_w_s, gm_b_s, cf_dw_w, cf_dw_b, cf_ln_g, cf_ln_b, cf_pw, pool_w,
           pool_scale, sc_w, mix_norm_g, w_o, ln1_g, ln1_b, router_w, router_b, exp_w_gate, exp_w_up,
           exp_w_down, ln2_g, ln2_b):
    f = lambda a: np.ascontiguousarray(np.asarray(a, dtype=np.float32))
    x = f(x)
    pp = np.zeros((L, 128, NPP), np.float32)
    bc3 = np.zeros((L, 128, 768), np.float32)
    lnp = np.zeros((L, 128, 4096), np.float32)
    gmwT = np.zeros((L, 128, 512), np.float32)
    pbd = np.zeros((L, 256, 128), np.float32)
    cf_dw_w, cf_dw_b, cf_ln_g, cf_ln_b = f(cf_dw_w), f(cf_dw_b), f(cf_ln_g), f(cf_ln_b)
    pool_scale, sc_w, mix_norm_g, gm_b_s = f(pool_scale), f(sc_w), f(mix_norm_g), f(gm_b_s)
    gm_w_s, pool_w = f(gm_w_s), f(pool_w)
    for l in range(L):
        for j in range(2):
            ch = slice(j * 128, (j + 1) * 128)
            pp[l, :, PP_DW + j * 31:PP_DW + (j + 1) * 31] = cf_dw_w[l][:, ch].T
            pp[l, :, PP_DWB + j] = cf_dw_b[l][ch]
            pp[l, :, PP_CLG + j] = cf_ln_g[l][ch]
            pp[l, :, PP_CLB + j] = cf_ln_b[l][ch]
            pp[l, :, PP_PSC + j] = pool_scale[l][ch]
            pp[l, :, PP_SCW + j * 3:PP_SCW + (j + 1) * 3] = sc_w[l][:, ch].T
        for c in range(6):
            pp[l, :, PP_MG + c] = mix_norm_g[l][256 + c * 128:256 + (c + 1) * 128]
        pp[l, :, PP_BS:PP_BS + 4] = gm_b_s[l].T
        bc3[l, :, 0:256] = f(gm_ln_g)[l][None, :]
        bc3[l, :, 256:512] = f(gm_ln_b)[l][None, :]
        bc3[l, :, 512:768] = mix_norm_g[l][None, 0:256]
        lnp[l, :, 0:1024] = f(ln1_g)[l][None, :]
        lnp[l, :, 1024:2048] = f(ln1_b)[l][None, :]
        lnp[l, :, 2048:3072] = f(ln2_g)[l][None, :]
        lnp[l, :, 3072:4096] = f(ln2_b)[l][None, :]
        for h in range(4):
            gmwT[l, :, h * 128:(h + 1) * 128] = gm_w_s[l, h].T
        for g in range(4):
            c, q = g // 2, g % 2
            pbd[l, c * 128 + q * 64:c * 128 + (q + 1) * 64, q * 64:(q + 1) * 64] = pool_w[l, g]
    rbb = np.ascontiguousarray(np.broadcast_to(f(router_b)[None, :], (128, NE))).astype(np.float32)
    shared = {
        "pp": pp, "bc3": bc3, "lnp": lnp, "gmwT": gmwT, "pbd": pbd, "rbb": rbb,
        "w_in": f(w_in), "w_o": f(w_o), "cf_pw": f(cf_pw), "router_w": f(router_w),
        "exp_w_gate": f(exp_w_gate), "exp_w_up": f(exp_w_up), "exp_w_down": f(exp_w_down),
    }
    in_maps = []
    for c in range(NCORES):
        bi, half = c // 2, c % 2
        xc = np.zeros((T, D), np.float32)
        xc[HALO:] = x[bi, half * TM:(half + 1) * TM]
        if half == 1:
            xc[:HALO] = x[bi, TM - HALO:TM]
        ident, maskT, rcfix, invw, hflag = _host_consts(half)
        m = dict(shared)
        m.update({"x": xc, "ident": ident, "maskT": maskT, "rcfix": rcfix, "invw": invw, "hflag": hflag})
        in_maps.append(m)
    if "nc" not in _CACHE:
        _CACHE["nc"] = build()
    res = run_bass_kernel_spmd(_CACHE["nc"], in_maps, core_ids=list(range(NCORES)))
    out = np.zeros((4, SEQ, D), np.float32)
    for c in range(NCORES):
        bi, half = c // 2, c % 2
        out[bi, half * TM:(half + 1) * TM] = res.results[c]["out"]
    return out
```

```python
from contextlib import ExitStack
import numpy as np
import concourse.bass as bass
import concourse.mybir as mybir
from concourse.bass_utils import run_bass_kernel_spmd

F32 = mybir.dt.float32
BF16 = mybir.dt.bfloat16
ALU = mybir.AluOpType
AF = mybir.ActivationFunctionType
AX = mybir.AxisListType
_DSZ = {F32: 4, BF16: 2}

L = 2
D = 1024
NCORES = 8
SEQ = 4096
TM = 2048
HALO = 128
T = TM + HALO
NT = T // 128
NE = 16
DE = 512
ALPHA = float((2 * L) ** 0.25)
LN_EPS = 1e-5
RMS_EPS = 1e-6
NB = 512
XNB = 512
XBLOCKS = [(0, 128)] + [(128 + i * XNB, XNB) for i in range(TM // XNB)]
MBLOCKS = [(0, 128)] + [(128 + i * NB, NB) for i in range(TM // NB)]
XOFF = []
_o = 0
for (_t0, _nb) in XBLOCKS:
    XOFF.append(_o)
    _o += 8 * _nb
XT_COLS = _o
NPP = 86
PP_DW = 0
PP_DWB = 62
PP_CLG = 64
PP_CLB = 66
PP_PSC = 68
PP_SCW = 70
PP_MG = 76
PP_BS = 82


class Ref:
    __slots__ = ("ap", "buf", "box")

    def __init__(self, ap, buf, box):
        self.ap = ap
        self.buf = buf
        self.box = box

    def v(self, fn):
        return Ref(fn(self.ap), self.buf, self.box)


class Buf:
    def __init__(self, handle, name, P, F, dtype):
        self.h = handle
        self.name = name
        self.P = P
        self.F = F
        self.dtype = dtype
        self.esz = _DSZ[dtype]
        self.entries = {}
        self.whole = False

    def __call__(self, f0=0, f1=None, p0=0, p1=None):
        if f1 is None:
            f1 = self.F
        if p1 is None:
            p1 = self.P
        assert 0 <= f0 < f1 <= self.F and 0 <= p0 < p1 <= self.P, (self.name, f0, f1, p0, p1)
        if self.whole:
            return Ref(self.h[p0:p1, f0:f1], self, (0, self.P, 0, self.F * self.esz))
        return Ref(self.h[p0:p1, f0:f1], self, (p0, p1, f0 * self.esz, f1 * self.esz))


def _ov(a, b):
    return a[0] < b[1] and b[0] < a[1] and a[2] < b[3] and b[2] < a[3]


def _cov(o, i):
    return o[0] <= i[0] and i[1] <= o[1] and o[2] <= i[2] and i[3] <= o[3]


class Eng:
    def __init__(self, name, eng):
        self.name = name
        self.e = eng
        self.sem = name
        self.ticket = 0
        self.seen = {}


class Emitter:
    def __init__(self, nc, sems):
        self.nc = nc
        self.sems = sems
        self.pe = Eng("pe", nc.tensor)
        self.act = Eng("act", nc.scalar)
        self.dve = Eng("dve", nc.vector)
        self.pool = Eng("pool", nc.gpsimd)
        self.sp = Eng("sp", nc.sync)
        self.dma_count = {}
        self.n_inst = 0
        self.n_wait = 0
        self.rec = None

    def _collect(self, E, reads, writes, is_dma):
        need = {}
        for r in reads:
            rb = r.box
            for (box, sk, w), val in r.buf.entries.items():
                if w and _ov(box, rb) and need.get(sk, 0) < val:
                    need[sk] = val
        for wr in writes:
            wb = wr.box
            for (box, sk, w), val in wr.buf.entries.items():
                if _ov(box, wb):
                    if sk == E.sem and E.name == "pe" and not is_dma:
                        continue
                    if need.get(sk, 0) < val:
                        need[sk] = val
        return need

    def _waits(self, E, need):
        for sk, val in need.items():
            if E.seen.get(sk, 0) >= val:
                continue
            E.e.wait_ge(self.sems[sk], val)
            E.seen[sk] = val
            self.n_wait += 1

    def _record(self, reads, writes, sk, val):
        for wr in writes:
            ent = wr.buf.entries
            wb = wr.box
            dead = [k for k in ent if _cov(wb, k[0])]
            for k in dead:
                del ent[k]
            ent[(wb, sk, True)] = val
        for r in reads:
            r.buf.entries[(r.box, sk, False)] = val

    def replay(self, rec):
        self.op(rec[0], rec[1], rec[2], rec[3])

    def op(self, E, fn, w=(), r=()):
        if self.rec is not None:
            self.rec.append((E, fn, tuple(w), tuple(r)))
            return None
        self._waits(E, self._collect(E, r, w, False))
        ins = fn(E.e)
        E.ticket += 1
        ins.then_inc(self.sems[E.sem], 1)
        self._record(r, w, E.sem, E.ticket)
        self.n_inst += 1
        return ins

    def dma(self, E, out, in_, semkey, w=(), r=(), group_left=0):
        self._waits(E, self._collect(E, r, w, True))
        ins = E.e.dma_start(out=out, in_=in_)
        c = self.dma_count.get(semkey, 0) + 16
        self.dma_count[semkey] = c
        ins.then_inc(self.sems[semkey], 16)
        self._record(r, w, semkey, c + 16 * group_left)
        self.n_inst += 1
        return ins


def build(n_layers=L, stop=None):
    nc = bass.Bass("TRN2", target_bir_lowering=False)

    def din(name, shape):
        return nc.dram_tensor(name, list(shape), F32, kind="ExternalInput").ap()

    x_d = din("x", [T, D])
    ident_d = din("ident", [128, 128])
    maskT_d = din("maskT", [128, 128])
    rcfix_d = din("rcfix", [128, 32])
    invw_d = din("invw", [128, 2])
    hflag_d = din("hflag", [128, 1])
    rbb_d = din("rbb", [128, NE])
    pp_d = din("pp", [L, 128, NPP])
    bc3_d = din("bc3", [L, 128, 768])
    lnp_d = din("lnp", [L, 128, 4096])
    gmwT_d = din("gmwT", [L, 128, 512])
    pbd_d = din("pbd", [L, 256, 128])
    w_in_d = din("w_in", [L, D, 2048])
    w_o_d = din("w_o", [L, D, D])
    cf_pw_d = din("cf_pw", [L, 256, 256])
    rw_d = din("router_w", [D, NE])
    wg_d = din("exp_w_gate", [L, NE, D, DE])
    wu_d = din("exp_w_up", [L, NE, D, DE])
    wd_d = din("exp_w_down", [L, NE, DE, D])
    out_d = nc.dram_tensor("out", [TM, D], F32, kind="ExternalOutput").ap()

    with ExitStack() as st:
        def sb(name, F, dt=F32, P=128):
            h = st.enter_context(nc.sbuf_tensor("s_" + name, [P, F], dt))
            return Buf(h, name, P, F, dt)

        def ps(name, F, dt=F32):
            h = st.enter_context(nc.psum_tensor(name, [128, F], dt))
            bf = Buf(h, name, 128, F, dt)
            bf.whole = True
            return bf

        semnames = ["pe", "act", "dve", "pool", "sp", "dx", "dc", "dpar", "dwin", "dwo",
                    "dwg0", "dwu0", "dwd0", "dwg1", "dwu1", "dwd1", "dout", "dln2", "dcp", "dparp"]
        sems = {k: st.enter_context(nc.semaphore(k)) for k in semnames}
        em = Emitter(nc, sems)
        PE, ACT, DVE, POOL, SP = em.pe, em.act, em.dve, em.pool, em.sp

        xtok = sb("xtok", NT * D)
        xT = sb("xT", XT_COLS, BF16)
        wbuf = sb("wbuf", 24576, BF16)
        lnp = sb("lnp", 2048)
        bc3 = sb("bc3", 768)
        ppb = sb("ppb", NPP)
        identf = sb("identf", 128)
        identb = sb("identb", 128, BF16)
        maskT = sb("maskT", 128)
        ones = sb("ones", 128)
        rcfix = sb("rcfix", 32)
        invw = sb("invw", 2)
        hflag = sb("hflag", 1)
        rbb = sb("rbb", NE)
        wmT = sb("wmT", 512, BF16)
        pwb = sb("pwb", 512, BF16)
        pbd = sb("pbd", 256, BF16)
        rwb = sb("rwb", 8 * NE, BF16)
        gtmp = sb("gtmp", 512)

        def gates(a=0, c=NT * NE):
            return gtmp(a, c)
        yT = sb("yT", 8 * NB, BF16)
        SW = NB + 32
        NS = 9
        ZL0, CH0, TM0 = 0, 2, 4
        scr = sb("scr", NS * SW)
        scrb = sb("scrb", 2 * 4 * XNB, BF16)
        HBW = 30 + NB + 2
        hbb = sb("hbb", 2 * HBW, BF16)
        dg = sb("dg", 4 * 128, BF16)
        sm = sb("sm", 384)

        def rt(a, c):
            return scr(2 * SW + a, 2 * SW + c)

        def SG(k, nb):
            return scr(k * SW, k * SW + nb)

        xbs = [(lambda a=0, c=D, k=k: scrb(2048 + k * D + a, 2048 + k * D + c)) for k in range(2)]

        def wmS(a=0, c=512):
            return scr(7 * SW + a, 7 * SW + c)

        ACC = [lambda a, c, p0=0, p1=128, k=k: scr((7 + k) * SW + a, (7 + k) * SW + c, p0, p1) for k in range(2)]

        def S(i, f0=0, f1=SW, p0=0, p1=128):
            return scr(i * SW + f0, i * SW + f1, p0, p1)

        B = [ps("pb%d" % i, 512) for i in range(6)]
        pTs = [ps("pT%d" % i, 1024, BF16) for i in range(2)]

        def mm(out, lhsT, rhs, start, stop):
            em.op(PE, lambda e: e.matmul(out.ap, lhsT.ap, rhs.ap, start=start, stop=stop), w=[out], r=[lhsT, rhs])

        def act(out, in_, func, bias=None, scale=None, extra_r=()):
            kw = {}
            if bias is not None:
                kw["bias"] = bias.ap if isinstance(bias, Ref) else bias
            if scale is not None:
                kw["scale"] = scale.ap if isinstance(scale, Ref) else scale
            rr = [in_] + [a for a in (bias, scale) if isinstance(a, Ref)] + list(extra_r)
            em.op(ACT, lambda e: e.activation(out=out.ap, in_=in_.ap, func=func, **kw), w=[out], r=rr)

        def tt(E, out, a, b, op):
            em.op(E, lambda e: e.tensor_tensor(out=out.ap, in0=a.ap, in1=b.ap, op=op), w=[out], r=[a, b])

        def ts(E, out, a, s1, op0, s2=None, op1=None):
            rr = [a] + [s for s in (s1, s2) if isinstance(s, Ref)]
            a1 = s1.ap if isinstance(s1, Ref) else s1
            a2 = s2.ap if isinstance(s2, Ref) else s2
            if op1 is None:
                em.op(E, lambda e: e.tensor_scalar(out=out.ap, in0=a.ap, scalar1=a1, scalar2=None, op0=op0), w=[out], r=rr)
            else:
                em.op(E, lambda e: e.tensor_scalar(out=out.ap, in0=a.ap, scalar1=a1, scalar2=a2, op0=op0, op1=op1),
                      w=[out], r=rr)

        def stt(E, out, a, s, b, op0, op1):
            rr = [a, b] + ([s] if isinstance(s, Ref) else [])
            sv = s.ap if isinstance(s, Ref) else s
            em.op(E, lambda e: e.scalar_tensor_tensor(out=out.ap, in0=a.ap, scalar=sv, in1=b.ap, op0=op0, op1=op1),
                  w=[out], r=rr)

        def cpy(E, out, in_):
            if E is ACT:
                em.op(E, lambda e: e.copy(out=out.ap, in_=in_.ap), w=[out], r=[in_])
            else:
                em.op(E, lambda e: e.tensor_copy(out=out.ap, in_=in_.ap), w=[out], r=[in_])

        def powm(out, in_, n):
            act(out, in_, AF.Ln)
            act(out, out, AF.Exp, scale=-0.5)

        def xloc(t0):
            if t0 < 128:
                return 0, t0
            return 1 + (t0 - 128) // XNB, (t0 - 128) % XNB

        def xT_tok(t0, n, kc):
            xb_, c0 = xloc(t0)
            nbx = XBLOCKS[xb_][1]
            assert c0 + n <= nbx
            return xT(XOFF[xb_] + kc * nbx + c0, XOFF[xb_] + kc * nbx + c0 + n)

        def xtile(i, f0=0, f1=D):
            return xtok(i * D + f0, i * D + f1)

        def pipeline(items, stages):
            n, m = len(items), len(stages)
            for step in range(n + m - 1):
                for k in range(m - 1, -1, -1):
                    idx = step - k
                    if 0 <= idx < n:
                        stages[k](idx, items[idx])

        def SMT(i, a, c):
            return sm(i * 20 + a, i * 20 + c)

        def ln_stages(goff, ring0=0):
            def sA(r, i):
                for c in range(2):
                    em.op(DVE, lambda e, c=c: e.bn_stats(out=SMT(i, c * 6, c * 6 + 6).ap, in_=xtile(i, c * 512, (c + 1) * 512).ap),
                          w=[SMT(i, c * 6, c * 6 + 6)], r=[xtile(i, c * 512, (c + 1) * 512)])
                em.op(DVE, lambda e: e.bn_aggr(out=SMT(i, 12, 14).ap, in_=SMT(i, 0, 12).ap), w=[SMT(i, 12, 14)],
                      r=[SMT(i, 0, 12)])
                ts(DVE, SMT(i, 14, 15), SMT(i, 13, 14), LN_EPS, ALU.add)

            def sB(r, i):
                powm(SMT(i, 15, 16), SMT(i, 14, 15), 1)
                stt(DVE, SMT(i, 16, 17), SMT(i, 12, 13), -1.0, SMT(i, 15, 16), ALU.mult, ALU.mult)

            def sC(r, i):
                act(xtile(i), xtile(i), AF.Identity, bias=SMT(i, 16, 17), scale=SMT(i, 15, 16))

            def sD(r, i):
                tt(POOL, xtile(i), xtile(i), lnp(goff, goff + D), ALU.mult)
                tt(POOL, xtile(i), xtile(i), lnp(goff + D, goff + 2 * D), ALU.add)
            return [sA, sB, sC, sD]

        def xT_stages(scale0=None, alpha_after=False, pt_fixed=None):
            def sE(r, i):
                xb = xbs[r % 2]
                if scale0 is not None and i == 0:
                    act(xb(), xtile(i), AF.Identity, scale=scale0)
                elif alpha_after:
                    act(xb(), xtile(i), AF.Identity, scale=1.0 / ALPHA)
                else:
                    cpy(ACT, xb(), xtile(i))

            def sF(r, i):
                xb = xbs[r % 2]
                pT = pTs[r % 2 if pt_fixed is None else pt_fixed]
                for kc in range(8):
                    em.op(PE, lambda e, kc=kc: e.transpose(out=pT(kc * 128, (kc + 1) * 128).ap, in_=xb(kc * 128, (kc + 1) * 128).ap,
                                                    identity=identb().ap),
                          w=[pT(kc * 128, (kc + 1) * 128)], r=[xb(kc * 128, (kc + 1) * 128), identb()])

            def sG(r, i):
                pT = pTs[r % 2 if pt_fixed is None else pt_fixed]
                b, c0 = xloc(i * 128)
                nb = XBLOCKS[b][1]
                dst = xT(XOFF[b], XOFF[b] + 8 * nb).v(
                    lambda ap: ap.rearrange("p (k t) -> p k t", k=8)[:, :, c0:c0 + 128])
                src = pT().v(lambda ap: ap.rearrange("p (k t) -> p k t", k=8))
                cpy(ACT, dst, src)
            return [sE, sF, sG]

        def rms_group(yf, nb, gcol, c0):
            sq = S(TM0 + 2, 0, nb)
            for j in range(2):
                act(sq, yf[j], AF.Square)
                mm(B[4](0, nb), ones(), sq, j == 0, j == 1)
            ve = S(TM0 + 1, 0, nb)
            ts(DVE, ve, B[4](0, nb), 1.0 / 256.0, ALU.mult, RMS_EPS, ALU.add)
            powm(ve, ve, nb)
            for j in range(2):
                stt(DVE, yT((c0 + j) * nb, (c0 + j + 1) * nb), yf[j], ppb(gcol + j, gcol + j + 1), ve, ALU.mult, ALU.mult)

        for i in range(NT):
            em.dma(SP, xtile(i).ap, x_d[i * 128:(i + 1) * 128, :], "dx", w=[xtile(i)], group_left=NT - 1 - i)
        cl = [(identf, ident_d), (maskT, maskT_d), (rcfix, rcfix_d), (invw, invw_d), (hflag, hflag_d), (rbb, rbb_d)]
        for n, (bf, d) in enumerate(cl):
            em.dma(SP, bf().ap, d, "dc", w=[bf()], group_left=len(cl) - 1 - n)
        em.dma(POOL, rwb().ap.rearrange("p (k n) -> p k n", k=8), rw_d.rearrange("(k p) n -> p k n", p=128), "dcp",
               w=[rwb()])
        cpy(ACT, identb(), identf())
        em.op(DVE, lambda e: e.memset(ones().ap, 1.0), w=[ones()])

        def load_layer_weights(l):
            for q in range(4):
                em.dma(POOL, wbuf(q * 4096, (q + 1) * 4096).ap.rearrange("p (k n) -> p k n", k=2),
                       w_in_d[l, q * 256:(q + 1) * 256, :].rearrange("(k p) n -> p k n", p=128), "dwin",
                       w=[wbuf(q * 4096, (q + 1) * 4096)], group_left=3 - q)
            for q in range(2):
                em.dma(POOL, wbuf(16384 + q * 4096, 16384 + (q + 1) * 4096).ap.rearrange("p (k n) -> p k n", k=4),
                       w_o_d[l, q * 512:(q + 1) * 512, :].rearrange("(k p) n -> p k n", p=128), "dwo",
                       w=[wbuf(16384 + q * 4096, 16384 + (q + 1) * 4096)], group_left=1 - q)

        def load_layer_params(l):
            lst = [(SP, ppb(), pp_d[l]), (SP, bc3(), bc3_d[l]), (SP, lnp(), lnp_d[l, :, 0:2048]), (SP, wmS(), gmwT_d[l])]
            n = len(lst)
            for k, (E, rf, d) in enumerate(lst):
                em.dma(E, rf.ap, d, "dpar", w=[rf], group_left=n - 1 - k)
            em.dma(POOL, pwb().ap.rearrange("p (k n) -> p k n", k=2),
                   cf_pw_d[l].rearrange("(k p) n -> p k n", p=128), "dparp", w=[pwb()], group_left=1)
            em.dma(POOL, pbd().ap.rearrange("p (k n) -> p k n", k=2),
                   pbd_d[l].rearrange("(k p) n -> p k n", p=128), "dparp", w=[pbd()], group_left=0)
            ts(DVE, lnp(), lnp(), ALPHA, ALU.mult)
            tt(DVE, wmT().v(lambda ap: ap.rearrange("p (h t) -> p h t", h=4)),
               wmS().v(lambda ap: ap.rearrange("p (h t) -> p h t", h=4)),
               maskT().v(lambda ap: ap.rearrange("p (o t) -> p o t", o=1).to_broadcast([128, 4, 128])), ALU.mult)

        def load_expert(l, e):
            s = e % 2
            base = s * 12288
            em.dma(POOL, wbuf(base, base + 4096).ap.rearrange("p (k n) -> p k n", k=8),
                   wg_d[l, e].rearrange("(k p) n -> p k n", p=128), "dwg%d" % s, w=[wbuf(base, base + 4096)])
            em.dma(POOL, wbuf(base + 4096, base + 8192).ap.rearrange("p (k n) -> p k n", k=8),
                   wu_d[l, e].rearrange("(k p) n -> p k n", p=128), "dwu%d" % s, w=[wbuf(base + 4096, base + 8192)])
            em.dma(POOL, wbuf(base + 8192, base + 12288).ap.rearrange("p (k n) -> p k n", k=4),
                   wd_d[l, e].rearrange("(k p) n -> p k n", p=128), "dwd%d" % s, w=[wbuf(base + 8192, base + 12288)])

        load_layer_weights(0)
        load_layer_params(0)
        pipeline(list(range(NT)), xT_stages())

        def win(kc, c0, c1):
            return wbuf(kc * 2048 + c0, kc * 2048 + c1)

        def wo(kc, c0, c1):
            return wbuf(16384 + kc * 1024 + c0, 16384 + kc * 1024 + c1)

        zrot = [0]
        dgi = [0]
        pre_e0 = [False]

        def zbank():
            b = B[2 + zrot[0] % 2]
            zrot[0] += 1
            return b

        def zmm(bank, t0, nb, col0):
            for kc in range(8):
                mm(bank(0, nb), win(kc, col0, col0 + 128), xT_tok(t0, nb, kc), kc == 0, kc == 7)

        def finish_dump():
            for i in range(1, NT):
                em.dma(SP, out_d[(i - 1) * 128:i * 128, :], xtile(i).ap, "dout", r=[xtile(i)])
            SP.e.wait_ge(sems["dout"], em.dma_count["dout"])
            build.stats = (em.n_inst, em.n_wait)

        if stop == "setup":
            finish_dump()
            return nc
        for l in range(n_layers):
            for j in range(2):
                em.op(DVE, lambda e: e.memset(hbb(j * HBW, j * HBW + 30).ap, 0.0), w=[hbb(j * HBW, j * HBW + 30)])
                em.op(DVE, lambda e: e.memset(S(ZL0 + j, 0, 15).ap, 0.0), w=[S(ZL0 + j, 0, 15)])
                em.op(DVE, lambda e: e.memset(S(CH0 + j, 0, 2).ap, 0.0), w=[S(CH0 + j, 0, 2)])

            last = (l == n_layers - 1)
            def G_section(b):
                t0, nb = MBLOCKS[b]
                tiles = list(range(t0 // 128, (t0 + nb) // 128))
                state_only = last and b == 0 and stop is None
                GS = 340

                def g_bufs(r):
                    p = r % 2
                    p = 0
                    pUV, pS_ = B[0], B[1]
                    sl = [(lambda a, c: gtmp(a, c)), (lambda a, c: gtmp(256 + a, 256 + c)), (lambda a, c: gtmp(a, c))]
                    u_sb, vn, yg = sl[0](0, 256), sl[1](0, 256), sl[2](0, 256)
                    q = lambda a, c: sm(GS + a, GS + c)
                    vb = lambda a, c: scrb(1024 + a, 1024 + c)
                    return pUV, pS_, sl, u_sb, vn, yg, q, vb, pTs[1]

                def g0(r, i):
                    pUV = g_bufs(r)[0]
                    for kc in range(8):
                        mm(pUV(), xT_tok(i * 128, 128, kc), win(kc, 0, 512), kc == 0, kc == 7)

                def g1(r, i):
                    pUV, pS_, sl, u_sb, vn, yg, q, vb, pT = g_bufs(r)
                    cpy(ACT, u_sb, pUV(0, 256))
                    cpy(ACT, vn, pUV(256, 512))

                def g2(r, i):
                    pUV, pS_, sl, u_sb, vn, yg, q, vb, pT = g_bufs(r)
                    em.op(DVE, lambda e: e.bn_stats(out=q(0, 6).ap, in_=vn.ap), w=[q(0, 6)], r=[vn])
                    em.op(DVE, lambda e: e.bn_aggr(out=q(6, 8).ap, in_=q(0, 6).ap), w=[q(6, 8)], r=[q(0, 6)])
                    ts(DVE, q(8, 9), q(7, 8), LN_EPS, ALU.add)

                def g3(r, i):
                    pUV, pS_, sl, u_sb, vn, yg, q, vb, pT = g_bufs(r)
                    powm(q(9, 10), q(8, 9), 1)

                def g4(r, i):
                    pUV, pS_, sl, u_sb, vn, yg, q, vb, pT = g_bufs(r)
                    ts(DVE, vn, vn, q(6, 7), ALU.subtract, q(9, 10), ALU.mult)
                    tt(DVE, vn, vn, bc3(0, 256), ALU.mult)
                    tt(DVE, vb(0, 256), vn, bc3(256, 512), ALU.add)

                def g5(r, i):
                    pUV, pS_, sl, u_sb, vn, yg, q, vb, pT = g_bufs(r)
                    for h in range(4):
                        mm(pS_(64 * h, 64 * h + 64), wmT(128 * h, 128 * h + 128), vb(64 * h, 64 * h + 64), True, True)

                def g6(r, i):
                    pUV, pS_, sl, u_sb, vn, yg, q, vb, pT = g_bufs(r)
                    for h in range(4):
                        stt(DVE, sl[2](64 * h, 64 * h + 64), pS_(64 * h, 64 * h + 64), ppb(PP_BS + h, PP_BS + h + 1),
                            sl[0](64 * h, 64 * h + 64), ALU.add, ALU.mult)
                    em.op(DVE, lambda e: e.bn_stats(out=q(10, 16).ap, in_=yg.ap), w=[q(10, 16)], r=[yg])
                    em.op(DVE, lambda e: e.bn_aggr(out=q(16, 18).ap, in_=q(10, 16).ap), w=[q(16, 18)], r=[q(10, 16)])
                    stt(DVE, q(18, 19), q(16, 17), q(16, 17), q(17, 18), ALU.mult, ALU.add)
                    ts(DVE, q(18, 19), q(18, 19), RMS_EPS, ALU.add)

                def g7(r, i):
                    pUV, pS_, sl, u_sb, vn, yg, q, vb, pT = g_bufs(r)
                    powm(q(19, 20), q(18, 19), 1)
                    stt(DVE, vb(256, 512), yg, q(19, 20), bc3(512, 768), ALU.mult, ALU.mult)

                def g8(r, i):
                    pUV, pS_, sl, u_sb, vn, yg, q, vb, pT = g_bufs(r)
                    for c in range(2):
                        em.op(PE, lambda e, c=c: e.transpose(out=pT(c * 128, (c + 1) * 128).ap,
                                                        in_=vb(256 + c * 128, 256 + (c + 1) * 128).ap,
                                                        identity=identb().ap),
                              w=[pT(c * 128, (c + 1) * 128)], r=[vb(256 + c * 128, 256 + (c + 1) * 128), identb()])

                def g9(r, i):
                    pT = g_bufs(r)[8]
                    c0 = i * 128 - t0
                    dst = yT(0, 2 * nb).v(lambda ap: ap.rearrange("p (k t) -> p k t", k=2)[:, :, c0:c0 + 128])
                    cpy(ACT, dst, pT(0, 256).v(lambda ap: ap.rearrange("p (k t) -> p k t", k=2)))

                for i_ in tiles:
                    for gs in [g0, g1, g2, g3, g4, g5, g6, g7, g8, g9]:
                        gs(0, i_)


            def CPS_section(b):
                t0, nb = MBLOCKS[b]
                tiles = list(range(t0 // 128, (t0 + nb) // 128))
                state_only = last and b == 0 and stop is None
                accs = []
                for j in range(2):
                    pa = zbank()
                    zmm(pa, t0, nb, 512 + j * 128)
                    pg = zbank()
                    zmm(pg, t0, nb, 768 + j * 128)
                    sig = S(TM0 + j, 0, nb)
                    act(sig, pg(0, nb), AF.Tanh, scale=0.5)
                    hb = lambda a, c, j=j: hbb(j * HBW + a, j * HBW + c)
                    stt(DVE, hb(30, 30 + nb), sig, 1.0, pa(0, nb), ALU.add, ALU.mult)
                hbs = [(lambda a, c, j=j: hbb(j * HBW + a, j * HBW + c)) for j in range(2)]
                if state_only:
                    for j in range(2):
                        cpy(ACT, hbs[j](0, 30), hbs[j](nb, nb + 30))
                else:
                    pcs = [zbank(), zbank()]
                    for k in range(31):
                        for j in range(2):
                            slot = dgi[0] % 4
                            dgi[0] += 1
                            wcol = ppb(PP_DW + j * 31 + k, PP_DW + j * 31 + k + 1)
                            GE = (POOL, ACT, POOL, ACT)[slot]
                            if GE is ACT:
                                act(dg(slot * 128, (slot + 1) * 128), identf(), AF.Identity, scale=wcol)
                            elif GE is DVE:
                                ts(GE, dg(slot * 128, (slot + 1) * 128), identf(), wcol, ALU.mult)
                            else:
                                ts(GE, dg(slot * 128, (slot + 1) * 128), identf(), wcol, ALU.mult, 0.0, ALU.add)
                            mm(pcs[j](0, nb), dg(slot * 128, (slot + 1) * 128), hbs[j](k, k + nb), k == 0, k == 30)
                    for j in range(2):
                        act(ACC[j](0, nb), pcs[j](0, nb), AF.Identity, bias=ppb(PP_DWB + j, PP_DWB + j + 1), scale=0.5)
                    for j in range(2):
                        cpy(ACT, hbs[j](0, 30), hbs[j](nb, nb + 30))
                        accs.append(ACC[j](0, nb))
                if state_only:
                    for j in range(2):
                        pz = zbank()
                        zmm(pz, t0, nb, 1024 + j * 128)
                        cpy(ACT, S(ZL0 + j, 15, 15 + nb), pz(0, nb))
                        cpy(ACT, S(ZL0 + j, 0, 15), S(ZL0 + j, nb, nb + 15))
                        pC_ = zbank()
                        zmm(pC_, t0, nb, 1536 + j * 128)
                        pH_ = zbank()
                        zmm(pH_, t0, nb, 1792 + j * 128)
                        cs = S(TM0, 0, nb)
                        cpy(ACT, cs, pC_(0, nb))
                        tt(DVE, S(CH0 + j, 2, 2 + nb), pH_(0, nb), cs, ALU.mult)
                        cpy(ACT, S(CH0 + j, 0, 2), S(CH0 + j, nb, nb + 2))
                    return
                sq = S(TM0 + 2, 0, nb)
                for j in range(2):
                    act(sq, accs[j], AF.Square)
                    mm(B[5](0, nb), ones(), accs[j], j == 0, j == 1)
                    mm(B[4](0, nb), ones(), sq, j == 0, j == 1)
                mean = S(TM0, 0, nb)
                ts(DVE, mean, B[5](0, nb), 1.0 / 256.0, ALU.mult)
                msq = S(TM0 + 1, 0, nb)
                tt(DVE, msq, mean, mean, ALU.mult)
                ve = S(TM0 + 2, 0, nb)
                ts(DVE, ve, B[4](0, nb), 1.0 / 256.0, ALU.mult, LN_EPS, ALU.add)
                tt(DVE, ve, ve, msq, ALU.subtract)
                powm(ve, ve, nb)
                for j in range(2):
                    tt(DVE, accs[j], accs[j], mean, ALU.subtract)
                    tt(DVE, accs[j], accs[j], ve, ALU.mult)
                    act(scrb(j * nb, (j + 1) * nb), accs[j], AF.Silu, bias=ppb(PP_CLB + j, PP_CLB + j + 1),
                        scale=ppb(PP_CLG + j, PP_CLG + j + 1))
                yf = []
                for jo in range(2):
                    for j in range(2):
                        mm(B[5](0, nb), pwb(j * 256 + jo * 128, j * 256 + (jo + 1) * 128), scrb(j * nb, (j + 1) * nb),
                           j == 0, j == 1)
                    cpy(ACT, ACC[jo](0, nb), B[5](0, nb))
                    yf.append(ACC[jo](0, nb))
                rms_group(yf, nb, PP_MG + 0, 2)

                zls = [(lambda a, c, p0=0, p1=128, j=j: S(ZL0 + j, a, c, p0, p1)) for j in range(2)]
                Xs = [(lambda a, c, p0=0, p1=128: S(TM0, a, c, p0, p1)), (lambda a, c, p0=0, p1=128: S(TM0 + 2, a, c, p0, p1))]
                Ys = [(lambda a, c, p0=0, p1=128: S(TM0 + 1, a, c, p0, p1)), ACC[1]]
                pls = [(lambda a, c, p0=0, p1=128, j=j: scrb(j * nb + a, j * nb + c, p0, p1)) for j in range(2)]
                pzb = [B[2], B[3]]
                pob = [B[5], B[4]]
                for j in range(2):
                    zmm(pzb[j], t0, nb, 1024 + j * 128)
                for j in range(2):
                    cpy(ACT, zls[j](15, 15 + nb), pzb[j](0, nb))
                for j in range(2):
                    tt(DVE, Xs[j](1, 15 + nb), zls[j](1, 15 + nb), zls[j](0, 14 + nb), ALU.add)
                for j in range(2):
                    tt(DVE, Ys[j](3, 15 + nb), Xs[j](3, 15 + nb), Xs[j](1, 13 + nb), ALU.add)
                tt(DVE, Xs[1](7, 15 + nb), Ys[1](7, 15 + nb), Ys[1](3, 11 + nb), ALU.add)
                tt(DVE, Ys[1](15, 15 + nb), Xs[1](15, 15 + nb), Xs[1](7, 7 + nb), ALU.add)
                for (p0, p1) in ((0, 64), (64, 128)):
                    for j in range(2):
                        src = Xs[j] if p0 == 0 else Ys[j]
                        stt(DVE, pls[j](0, nb, p0, p1), src(15, 15 + nb, p0, p1), invw(j, j + 1, p0, p1),
                            zls[j](15, 15 + nb, p0, p1), ALU.mult, ALU.subtract)
                if b == 1:
                    for j in range(2):
                        for (p0, p1) in ((0, 64), (64, 128)):
                            src = Xs[j] if p0 == 0 else Ys[j]
                            tmp = sm(368, 384, p0, p1)
                            tt(DVE, tmp, src(15, 31, p0, p1), rcfix(j * 16, j * 16 + 16, p0, p1), ALU.mult)
                            tt(DVE, pls[j](0, 16, p0, p1), tmp, zls[j](15, 31, p0, p1), ALU.subtract)
                for j in range(2):
                    cpy(ACT, zls[j](0, 15), zls[j](nb, nb + 15))
                for j in range(2):
                    mm(pob[j](0, nb), pbd(j * 128, (j + 1) * 128), pls[j](0, nb), True, True)
                for j in range(2):
                    act(ACC[j](0, nb), pob[j](0, nb), AF.Identity, scale=ppb(PP_PSC + j, PP_PSC + j + 1))
                rms_group([ACC[0](0, nb), ACC[1](0, nb)], nb, PP_MG + 2, 4)

                def zmm_to(bank, col0):
                    zmm(bank, t0, nb, col0)
                    return bank
                sbank = [(B[2], B[3], B[2]), (B[4], B[5], B[4])]
                chls = [(lambda a, c, j=j: S(CH0 + j, a, c)) for j in range(2)]
                css = [S(TM0 + j, 0, nb) for j in range(2)]
                for j in range(2):
                    zmm_to(sbank[j][0], 1536 + j * 128)
                    zmm_to(sbank[j][1], 1792 + j * 128)
                for j in range(2):
                    cpy(ACT, css[j], sbank[j][0](0, nb))
                for j in range(2):
                    tt(DVE, chls[j](2, 2 + nb), sbank[j][1](0, nb), css[j], ALU.mult)
                for j in range(2):
                    zmm_to(sbank[j][2], 1280 + j * 128)
                for j in range(2):
                    ts(DVE, ACC[j](0, nb), chls[j](0, nb), ppb(PP_SCW + j * 3, PP_SCW + j * 3 + 1), ALU.mult)
                for k in (1, 2):
                    for j in range(2):
                        stt(DVE, ACC[j](0, nb), chls[j](k, k + nb), ppb(PP_SCW + j * 3 + k, PP_SCW + j * 3 + k + 1),
                            ACC[j](0, nb), ALU.mult, ALU.add)
                for j in range(2):
                    tt(DVE, ACC[j](0, nb), sbank[j][2](0, nb), ACC[j](0, nb), ALU.mult)
                for j in range(2):
                    cpy(ACT, chls[j](0, 2), chls[j](nb, nb + 2))
                rms_group([ACC[0](0, nb), ACC[1](0, nb)], nb, PP_MG + 4, 6)


            def O0_section(b):
                t0, nb = MBLOCKS[b]
                tiles = list(range(t0 // 128, (t0 + nb) // 128))
                for r, i in enumerate(tiles):
                    c0 = i * 128 - t0
                    banks = (B[0], B[1]) if r % 2 == 0 else (B[2], B[3])
                    for nh in range(2):
                        for kc in range(8):
                            mm(banks[nh](), yT(kc * nb + c0, kc * nb + c0 + 128), wo(kc, nh * 512, (nh + 1) * 512),
                               kc == 0, kc == 7)
                        stt(DVE, xtile(i, nh * 512, (nh + 1) * 512), xtile(i, nh * 512, (nh + 1) * 512), ALPHA, banks[nh](),
                            ALU.mult, ALU.add)

            def A_section(b):
                t0, nb = MBLOCKS[b]
                tiles = list(range(t0 // 128, (t0 + nb) // 128))
                pipeline(tiles, ln_stages(0) + xT_stages(alpha_after=True, pt_fixed=0))

            def recorded(fn, *a):
                em.rec = []
                fn(*a)
                r_, em.rec = em.rec, None
                return r_

            def interleave(ra, rb):
                na, nb_ = len(ra), len(rb)
                out, ia, ib = [], 0, 0
                while ia < na or ib < nb_:
                    if ib >= nb_ or (ia < na and ia * nb_ <= ib * na):
                        out.append(ra[ia]); ia += 1
                    else:
                        out.append(rb[ib]); ib += 1
                return out

            nblk = len(MBLOCKS)
            skip0 = last and stop is None
            if skip0:
                CPS_section(0)
            fb = 1 if skip0 else 0
            G_section(fb)
            CPS_section(fb)
            O0_section(fb)
            for b in range(fb, nblk):
                ra = recorded(A_section, b)
                if b + 1 < nblk:
                    x = interleave(ra, recorded(G_section, b + 1))
                    for rec in interleave(x, recorded(CPS_section, b + 1)):
                        em.replay(rec)
                    O0_section(b + 1)
                    pass
                else:
                    for rec in ra:
                        em.replay(rec)
            if stop == "mixer":
                finish_dump()
                return nc
            em.dma(SP, lnp().ap, lnp_d[l, :, 2048:4096], "dln2", w=[lnp()])
            if not pre_e0[0]:
                load_expert(l, 0)
            pre_e0[0] = False
            load_expert(l, 1)
            NL = NT * NE
            for i in range(NT):
                for kc in range(8):
                    mm(B[4](i * NE, (i + 1) * NE), xT_tok(i * 128, 128, kc), rwb(kc * NE, (kc + 1) * NE), kc == 0, kc == 7)
            lg = rt(0, NL)
            sel = rt(NL, 2 * NL)
            msk = rt(2 * NL, 3 * NL)
            eq1 = rt(3 * NL, 4 * NL)
            eq2 = rt(4 * NL, 5 * NL)
            tmp = rt(5 * NL, 6 * NL)
            pr = rt(3 * NL, 3 * NL + 68 * 6)
            o = 6 * NL
            gs = rt(o, o + 68)
            gmax = rt(o + 68, o + 85)
            m1 = rt(o + 85, o + 102)
            la = rt(o + 102, o + 119)
            lb = rt(o + 119, o + 136)
            g1 = rt(o + 136, o + 153)
            g2 = rt(o + 153, o + 170)
            v4 = lambda rf: rf.v(lambda ap: ap.rearrange("p (g f) -> p g f", f=4))
            v16 = lambda rf: rf.v(lambda ap: ap.rearrange("p (t e) -> p t e", e=NE))
            bc16 = lambda rf: rf.v(lambda ap: ap.rearrange("p (t o) -> p t o", o=1).to_broadcast([128, NT, NE]))
            cpy(DVE, lg, B[4](0, NL))
            tt(DVE, v16(sel), v16(lg),
               rbb().v(lambda ap: ap.rearrange("p (o e) -> p o e", o=1).to_broadcast([128, NT, NE])), ALU.add)
            s4v = sel.v(lambda ap: ap.rearrange("p (g f) -> p g f", f=4))
            p6 = lambda a, c: pr.v(lambda ap: ap.rearrange("p (g f) -> p g f", f=6)[:, :, a:c])
            s4s = lambda a, c: sel.v(lambda ap: ap.rearrange("p (g f) -> p g f", f=4)[:, :, a:c])
            tt(DVE, p6(0, 3), s4s(0, 3), s4s(1, 4), ALU.add)
            tt(DVE, p6(3, 5), s4s(0, 2), s4s(2, 4), ALU.add)
            tt(DVE, p6(5, 6), s4s(0, 1), s4s(3, 4), ALU.add)
            em.op(DVE, lambda e: e.reduce_max(out=gs.ap, in_=pr.ap.rearrange("p (g f) -> p g f", f=6), axis=AX.X),
                  w=[gs], r=[pr])
            em.op(DVE, lambda e: e.reduce_max(out=gmax.ap, in_=gs.ap.rearrange("p (t g) -> p t g", g=4), axis=AX.X),
                  w=[gmax], r=[gs])
            ing = rt(o + 170, o + 238)
            tt(DVE, ing.v(lambda ap: ap.rearrange("p (t g) -> p t g", g=4)),
               gs.v(lambda ap: ap.rearrange("p (t g) -> p t g", g=4)),
               gmax.v(lambda ap: ap.rearrange("p (t o) -> p t o", o=1).to_broadcast([128, NT, 4])), ALU.is_equal)
            ts(DVE, ing, ing, 1.0, ALU.subtract, 1e30, ALU.mult)
            tt(DVE, v4(msk), v4(sel),
               ing.v(lambda ap: ap.rearrange("p (g o) -> p g o", o=1).to_broadcast([128, 68, 4])), ALU.add)
            em.op(DVE, lambda e: e.reduce_max(out=m1.ap, in_=msk.ap.rearrange("p (t e) -> p t e", e=NE), axis=AX.X),
                  w=[m1], r=[msk])
            tt(DVE, v16(eq1), v16(msk), bc16(m1), ALU.is_equal)
            stt(DVE, msk, eq1, -1e30, msk, ALU.mult, ALU.add)
            em.op(DVE, lambda e: e.reduce_max(out=m1.ap, in_=msk.ap.rearrange("p (t e) -> p t e", e=NE), axis=AX.X),
                  w=[m1], r=[msk])
            tt(DVE, v16(eq2), v16(msk), bc16(m1), ALU.is_equal)
            tt(DVE, tmp, eq1, lg, ALU.mult)
            em.op(DVE, lambda e: e.reduce_sum(out=la.ap, in_=tmp.ap.rearrange("p (t e) -> p t e", e=NE), axis=AX.X),
                  w=[la], r=[tmp])
            tt(DVE, tmp, eq2, lg, ALU.mult)
            em.op(DVE, lambda e: e.reduce_sum(out=lb.ap, in_=tmp.ap.rearrange("p (t e) -> p t e", e=NE), axis=AX.X),
                  w=[lb], r=[tmp])
            tt(DVE, la, la, lb, ALU.subtract)
            act(g1, la, AF.Sigmoid)
            act(g2, la, AF.Sigmoid, scale=-1.0)
            tt(DVE, v16(eq1), v16(eq1), bc16(g1), ALU.mult)
            tt(DVE, v16(eq2), v16(eq2), bc16(g2), ALU.mult)
            tt(DVE, gates(), eq1, eq2, ALU.add)
            if stop == "router":
                finish_dump()
                return nc

            def ln2_block(tl):
                if l + 1 < n_layers:
                    pipeline(tl, ln_stages(0) + xT_stages(scale0=hflag()))
                else:
                    def sOut(r, i):
                        if i >= 1:
                            em.dma(SP, out_d[(i - 1) * 128:i * 128, :], xtile(i).ap, "dout", r=[xtile(i)])
                    pipeline(tl, ln_stages(0) + [sOut])

            pend_ln = []
            ln2_st = ln_stages(0)

            def ln2_out(r, i):
                if i >= 1:
                    em.dma(SP, out_d[(i - 1) * 128:i * 128, :], xtile(i).ap, "dout", r=[xtile(i)])

            it = 0
            for e_ in range(NE):
                spread_ln = (e_ == NE - 1) and stop is None
                s = e_ % 2
                base = s * 12288
                for b, (t0, nb) in enumerate(XBLOCKS):
                    if last and b == 0 and stop is None:
                        continue
                    tiles = list(range(t0 // 128, (t0 + nb) // 128))
                    hb0 = (it % 2) * 4 * XNB
                    for fc in range(4):
                        pG = B[(fc % 2) * 2]
                        pU = B[(fc % 2) * 2 + 1]
                        for kc in range(8):
                            mm(pG(0, nb), wbuf(base + kc * 512 + fc * 128, base + kc * 512 + (fc + 1) * 128),
                               xT_tok(t0, nb, kc), kc == 0, kc == 7)
                        for kc in range(8):
                            mm(pU(0, nb), wbuf(base + 4096 + kc * 512 + fc * 128, base + 4096 + kc * 512 + (fc + 1) * 128),
                               xT_tok(t0, nb, kc), kc == 0, kc == 7)
                        sg = SG(fc % 2, nb)
                        act(sg, pG(0, nb), AF.Silu)
                        tt(DVE, scrb(hb0 + fc * nb, hb0 + (fc + 1) * nb), sg, pU(0, nb), ALU.mult)
                        if spread_ln and pend_ln:
                            for r_, i_ in enumerate(pend_ln[0]):
                                ln2_st[fc](r_, i_)
                    if spread_ln and pend_ln:
                        for r_, i_ in enumerate(pend_ln.pop()):
                            if last:
                                ln2_out(r_, i_)
                    for i in tiles:
                        c0 = i * 128 - t0
                        for nh in range(2):
                            pY = B[4 + nh]
                            for fc in range(4):
                                mm(pY(), scrb(hb0 + fc * nb + c0, hb0 + fc * nb + c0 + 128),
                                   wbuf(base + 8192 + fc * 1024 + nh * 512, base + 8192 + fc * 1024 + (nh + 1) * 512),
                                   fc == 0, fc == 3)
                            stt(DVE, xtile(i, nh * 512, (nh + 1) * 512), pY(), gates(i * NE + e_, i * NE + e_ + 1),
                                xtile(i, nh * 512, (nh + 1) * 512), ALU.mult, ALU.add)
                    it += 1
                    if spread_ln:
                        pend_ln.append(tiles)
                if stop == "e%d" % e_:
                    finish_dump()
                    return nc
                if e_ + 2 < NE:
                    load_expert(l, e_ + 2)
            if l + 1 < n_layers:
                load_layer_weights(l + 1)
            while pend_ln:
                tl = pend_ln.pop()
                if last:
                    ln2_block(tl)
                else:
                    pipeline(tl, ln_stages(0))
            if stop is None and not last:
                pipeline(list(range(NT)), xT_stages(scale0=hflag()))
            if stop is not None:
                ln2_block([i for i in range(NT)])
            if l + 1 < n_layers:
                load_layer_params(l + 1)

        SP.e.wait_ge(sems["dout"], em.dma_count["dout"])
        build.stats = (em.n_inst, em.n_wait)
    return nc


_CACHE = {}


def _host_consts(half):
    ident = np.eye(128, dtype=np.float32)
    s = np.arange(128)
    maskT = (s[:, None] <= s[None, :]).astype(np.float32)
    wins = np.array([2, 4, 8, 16])
    p = np.arange(128)
    rcfix = np.zeros((128, 32), np.float32)
    invw = np.zeros((128, 2), np.float32)
    for j in range(2):
        w = wins[2 * j + (p // 64)].astype(np.float32)
        invw[:, j] = 1.0 / w
        t = np.arange(16, dtype=np.float32)[None, :]
        if half == 0:
            rcfix[:, j * 16:(j + 1) * 16] = 1.0 / np.minimum(t + 1.0, w[:, None])
        else:
            rcfix[:, j * 16:(j + 1) * 16] = 1.0 / w[:, None]
    hflag = np.full((128, 1), float(half), np.float32)
    return ident, maskT, rcfix, invw, hflag


def kernel(x, w_in, gm_ln_g, gm_ln_b, gm_w_s, gm_b_s, cf_dw_w, cf_dw_b, cf_ln_g, cf_ln_b, cf_pw, pool_w,
           pool_scale, sc_w, mix_norm_g, w_o, ln1_g, ln1_b, router_w, router_b, exp_w_gate, exp_w_up,
           exp_w_down, ln2_g, ln2_b):
    f = lambda a: np.ascontiguousarray(np.asarray(a, dtype=np.float32))
    x = f(x)
    pp = np.zeros((L, 128, NPP), np.float32)
    bc3 = np.zeros((L, 128, 768), np.float32)
    lnp = np.zeros((L, 128, 4096), np.float32)
    gmwT = np.zeros((L, 128, 512), np.float32)
    pbd = np.zeros((L, 256, 128), np.float32)
    cf_dw_w, cf_dw_b, cf_ln_g, cf_ln_b = f(cf_dw_w), f(cf_dw_b), f(cf_ln_g), f(cf_ln_b)
    pool_scale, sc_w, mix_norm_g, gm_b_s = f(pool_scale), f(sc_w), f(mix_norm_g), f(gm_b_s)
    gm_w_s, pool_w = f(gm_w_s), f(pool_w)
    for l in range(L):
        for j in range(2):
            ch = slice(j * 128, (j + 1) * 128)
            pp[l, :, PP_DW + j * 31:PP_DW + (j + 1) * 31] = cf_dw_w[l][:, ch].T
            pp[l, :, PP_DWB + j] = cf_dw_b[l][ch]
            pp[l, :, PP_CLG + j] = cf_ln_g[l][ch]
            pp[l, :, PP_CLB + j] = cf_ln_b[l][ch]
            pp[l, :, PP_PSC + j] = pool_scale[l][ch]
            pp[l, :, PP_SCW + j * 3:PP_SCW + (j + 1) * 3] = sc_w[l][:, ch].T
        for c in range(6):
            pp[l, :, PP_MG + c] = mix_norm_g[l][256 + c * 128:256 + (c + 1) * 128]
        pp[l, :, PP_BS:PP_BS + 4] = gm_b_s[l].T
        bc3[l, :, 0:256] = f(gm_ln_g)[l][None, :]
        bc3[l, :, 256:512] = f(gm_ln_b)[l][None, :]
        bc3[l, :, 512:768] = mix_norm_g[l][None, 0:256]
        lnp[l, :, 0:1024] = f(ln1_g)[l][None, :]
        lnp[l, :, 1024:2048] = f(ln1_b)[l][None, :]
        lnp[l, :, 2048:3072] = f(ln2_g)[l][None, :]
        lnp[l, :, 3072:4096] = f(ln2_b)[l][None, :]
        for h in range(4):
            gmwT[l, :, h * 128:(h + 1) * 128] = gm_w_s[l, h].T
        for g in range(4):
            c, q = g // 2, g % 2
            pbd[l, c * 128 + q * 64:c * 128 + (q + 1) * 64, q * 64:(q + 1) * 64] = pool_w[l, g]
    rbb = np.ascontiguousarray(np.broadcast_to(f(router_b)[None, :], (128, NE))).astype(np.float32)
    shared = {
        "pp": pp, "bc3": bc3, "lnp": lnp, "gmwT": gmwT, "pbd": pbd, "rbb": rbb,
        "w_in": f(w_in), "w_o": f(w_o), "cf_pw": f(cf_pw), "router_w": f(router_w),
        "exp_w_gate": f(exp_w_gate), "exp_w_up": f(exp_w_up), "exp_w_down": f(exp_w_down),
    }
    in_maps = []
    for c in range(NCORES):
        bi, half = c // 2, c % 2
        xc = np.zeros((T, D), np.float32)
        xc[HALO:] = x[bi, half * TM:(half + 1) * TM]
        if half == 1:
            xc[:HALO] = x[bi, TM - HALO:TM]
        ident, maskT, rcfix, invw, hflag = _host_consts(half)
        m = dict(shared)
        m.update({"x": xc, "ident": ident, "maskT": maskT, "rcfix": rcfix, "invw": invw, "hflag": hflag})
        in_maps.append(m)
    if "nc" not in _CACHE:
        _CACHE["nc"] = build()
    res = run_bass_kernel_spmd(_CACHE["nc"], in_maps, core_ids=list(range(NCORES)))
    out = np.zeros((4, SEQ, D), np.float32)
    for c in range(NCORES):
        bi, half = c // 2, c % 2
        out[bi, half * TM:(half + 1) * TM] = res.results[c]["out"]
    return out
```
